# Optimizing a Trainium2 kernel written in Bass

```python
import math
import jax, jax.numpy as jnp
from jax import lax
import numpy as np

D_MODEL = 1024
BATCH = 2
SEQ = 16384
DEPTH = 1

N_MEM = 256

NSA_WIDTH = D_MODEL // 2
HG_WIDTH = D_MODEL - NSA_WIDTH

NSA_HEAD_DIM = 64
NSA_HEADS = NSA_WIDTH // NSA_HEAD_DIM
NSA_KV_GROUPS = 2
NSA_HPG = NSA_HEADS // NSA_KV_GROUPS
CMP_BLOCK = 32
CMP_STRIDE = 16
CMP_HIDDEN = 256
SEL_BLOCK = 64
SEL_TOP_N = 16
WINDOW = 512
Q_BLOCK = 128
FORCE_BONUS = 1.0e4

HG_KEY_DIM = 128
HG_VAL_DIM = 128
HG_HEADS = HG_WIDTH // HG_VAL_DIM
HG_CHUNK = 64

XA_HEADS = 4
XA_HEAD_DIM = D_MODEL // XA_HEADS

MOE_GROUPS = 4
MOE_EXPERTS_PER_GROUP = 8
MOE_N_EXPERTS = MOE_GROUPS * MOE_EXPERTS_PER_GROUP
MOE_TOP_K = 2
MOE_D_FF = 512
MOE_BLOCK = 128

DEEPNORM_ALPHA = (2.0 * DEPTH) ** 0.25
DEEPNORM_BETA = (8.0 * DEPTH) ** -0.25

LN_EPS = 1e-5
RMS_EPS = 1e-6
NEG_INF = -1e30

NSA_Q_COLS = NSA_HEADS * NSA_HEAD_DIM
NSA_KV_COLS = NSA_KV_GROUPS * NSA_HEAD_DIM
NSA_GATE_COLS = NSA_HEADS * 3
HG_QF_COLS = HG_HEADS * HG_KEY_DIM
HG_IV_COLS = HG_HEADS * HG_VAL_DIM
IN_SPLITS = (NSA_Q_COLS, NSA_KV_COLS, NSA_KV_COLS, NSA_KV_COLS, NSA_KV_COLS, NSA_KV_COLS, NSA_KV_COLS,
             NSA_GATE_COLS, HG_QF_COLS, HG_QF_COLS, HG_IV_COLS, HG_IV_COLS)
VALUE_SPLITS = (False, False, True, False, True, False, True, False, False, False, True, False)
IN_COLS = NSA_Q_COLS + 6 * NSA_KV_COLS + NSA_GATE_COLS + 2 * HG_QF_COLS + 2 * HG_IV_COLS

kernel_name = "hybrid_nsa_hgrn2_hmoe_layer"


def layer_norm(x, g, b):
    xf = x.astype(jnp.float32)
    mu = jnp.mean(xf, axis=-1, keepdims=True)
    var = jnp.mean(jnp.square(xf - mu), axis=-1, keepdims=True)
    return ((xf - mu) * lax.rsqrt(var + LN_EPS) * g + b).astype(x.dtype)


def rms_norm(x, g):
    xf = x.astype(jnp.float32)
    return xf * lax.rsqrt(jnp.mean(xf * xf, axis=-1, keepdims=True) + RMS_EPS) * g


def masked_softmax(s, mask):
    s = jnp.where(mask, s.astype(jnp.float32), NEG_INF)
    p = jax.nn.softmax(s, axis=-1)
    return jnp.where(mask, p, 0.0)


def split_cols(h):
    offs = np.cumsum((0,) + IN_SPLITS)
    return [h[..., int(offs[i]):int(offs[i + 1])] for i in range(len(IN_SPLITS))]


def nsa_compress(k, pe, w1, w2):
    b, s, g, d = k.shape
    n_sub = CMP_BLOCK // CMP_STRIDE
    n_ch = s // CMP_STRIDE
    ncmp = n_ch - n_sub + 1
    ch = k.reshape(b, n_ch, CMP_STRIDE, g, d)
    blocks = jnp.concatenate([ch[:, i:i + ncmp] for i in range(n_sub)], axis=2)
    blocks = blocks + pe[None, None, :, None, :]
    blocks = jnp.moveaxis(blocks, 3, 1).reshape(b, g, ncmp, CMP_BLOCK * d)
    return jax.nn.gelu(blocks @ w1) @ w2


def cmp_to_sel(p, n_sel):
    n_sub = CMP_BLOCK // CMP_STRIDE
    r = SEL_BLOCK // CMP_STRIDE
    ncmp = p.shape[-1]
    pp = jnp.pad(p, [(0, 0)] * (p.ndim - 1) + [(n_sub - 1, r * n_sel - ncmp)])
    return sum(pp[..., m:m + r * n_sel:r] for m in range(r + n_sub - 1))


def nsa_attention(q, kc, vc, ks, vs, kw, vw, gates):
    b, h, s, d = q.shape
    g = NSA_KV_GROUPS
    ncmp = kc.shape[2]
    n_sel = s // SEL_BLOCK
    top_n = min(SEL_TOP_N, n_sel)
    scale = d ** -0.5
    kw_pad = jnp.pad(kw, ((0, 0), (0, 0), (WINDOW, 0), (0, 0)))
    vw_pad = jnp.pad(vw, ((0, 0), (0, 0), (WINDOW, 0), (0, 0)))
    cmp_end = jnp.arange(ncmp) * CMP_STRIDE + CMP_BLOCK - 1
    blk_ids = jnp.arange(n_sel)
    bi = jnp.arange(b)[:, None, None, None]
    gi = jnp.arange(g)[None, :, None, None]
    offs = jnp.arange(SEL_BLOCK)

    def one_block(qb):
        s0 = qb * Q_BLOCK
        t = s0 + jnp.arange(Q_BLOCK)
        qq = lax.dynamic_slice_in_dim(q, s0, Q_BLOCK, axis=2)
        qq = qq.reshape(b, g, NSA_HPG, Q_BLOCK, d) * scale
        sc = jnp.einsum('bgrtd,bgnd->bgrtn', qq, kc)
        pc = masked_softmax(sc, cmp_end[None, :] <= t[:, None])
        o_c = jnp.einsum('bgrtn,bgnd->bgrtd', pc.astype(vc.dtype), vc)
        imp = cmp_to_sel(jnp.sum(pc, axis=2), n_sel)
        cur = t // SEL_BLOCK
        valid = blk_ids[None, :] <= cur[:, None]
        forced = ((blk_ids[None, :] == 0) | (blk_ids[None, :] == cur[:, None])
                  | (blk_ids[None, :] == cur[:, None] - 1))
        score = jnp.where(valid, imp + jnp.where(forced, FORCE_BONUS, 0.0), -1.0)
        _, idx = lax.top_k(score, top_n)
        pos = (idx[..., None] * SEL_BLOCK + offs).reshape(b, g, Q_BLOCK, top_n * SEL_BLOCK)
        k_sel = ks[bi, gi, pos]
        v_sel = vs[bi, gi, pos]
        ss = jnp.einsum('bgrtd,bgtkd->bgrtk', qq, k_sel)
        ps = masked_softmax(ss, (pos <= t[None, None, :, None])[:, :, None])
        o_s = jnp.einsum('bgrtk,bgtkd->bgrtd', ps.astype(v_sel.dtype), v_sel)
        kwb = lax.dynamic_slice_in_dim(kw_pad, s0, WINDOW + Q_BLOCK, axis=2)
        vwb = lax.dynamic_slice_in_dim(vw_pad, s0, WINDOW + Q_BLOCK, axis=2)
        kpos = s0 - WINDOW + jnp.arange(WINDOW + Q_BLOCK)
        m_w = ((kpos[None, :] <= t[:, None]) & (kpos[None, :] > t[:, None] - WINDOW)
               & (kpos[None, :] >= 0))
        sw = jnp.einsum('bgrtd,bgkd->bgrtk', qq, kwb)
        pw = masked_softmax(sw, m_w)
        o_w = jnp.einsum('bgrtk,bgkd->bgrtd', pw.astype(vwb.dtype), vwb)
        gb = lax.dynamic_slice_in_dim(gates, s0, Q_BLOCK, axis=2).reshape(b, g, NSA_HPG, Q_BLOCK, 3)
        o = gb[..., 0:1] * o_c + gb[..., 1:2] * o_s + gb[..., 2:3] * o_w
        return o.reshape(b, h, Q_BLOCK, d)

    out = lax.map(one_block, jnp.arange(s // Q_BLOCK))
    return jnp.transpose(out, (1, 0, 3, 2, 4)).reshape(b, s, h * d)


def hgrn2_recurrence(q, f_logit, i, lb):
    b, s, h, dk = q.shape
    dv = i.shape[-1]
    lb = lb.reshape(h, dk)
    log_f = jnp.logaddexp(jnp.log(lb), jnp.log1p(-lb) + jax.nn.log_sigmoid(f_logit.astype(jnp.float32)))
    k = -jnp.expm1(log_f)
    nc = s // HG_CHUNK

    def to_chunks(a):
        return jnp.transpose(a.astype(jnp.float32).reshape(b, nc, HG_CHUNK, h, a.shape[-1]), (1, 0, 3, 2, 4))

    qc, kc, vc, lc = to_chunks(q), to_chunks(k), to_chunks(i), to_chunks(log_f)
    bc = jnp.cumsum(lc, axis=3)
    causal = jnp.tril(jnp.ones((HG_CHUNK, HG_CHUNK), dtype=bool))

    def step(state, xs):
        qt, kt, vt, bt = xs
        inter = jnp.einsum('bhtk,bhkv->bhtv', qt * jnp.exp(bt), state)
        diff = bt[:, :, :, None, :] - bt[:, :, None, :, :]
        decay = jnp.where(causal[:, :, None], jnp.exp(jnp.minimum(diff, 0.0)), 0.0)
        att = jnp.einsum('bhtk,bhsk,bhtsk->bhts', qt, kt, decay)
        intra = jnp.einsum('bhts,bhsv->bhtv', att, vt)
        b_last = bt[:, :, -1:, :]
        new_state = (jnp.exp(b_last[:, :, 0, :])[..., None] * state
                     + jnp.einsum('bhsk,bhsv->bhkv', kt * jnp.exp(b_last - bt), vt))
        return new_state, inter + intra

    s_init = jnp.zeros((b, h, dk, dv), jnp.float32)
    _, o = lax.scan(step, s_init, (qc, kc, vc, bc))
    return jnp.transpose(o, (1, 0, 3, 2, 4)).reshape(b, s, h, dv)


def memory_cross_attention(x, mem, wq, wk, wv, wo):
    b, s, d = x.shape
    m = mem.shape[1]
    q = (x @ wq).reshape(b, s, XA_HEADS, XA_HEAD_DIM)
    k = (mem @ wk).reshape(b, m, XA_HEADS, XA_HEAD_DIM)
    v = (mem @ wv).reshape(b, m, XA_HEADS, XA_HEAD_DIM)
    sc = jnp.einsum('bshd,bmhd->bhsm', q, k).astype(jnp.float32) * (XA_HEAD_DIM ** -0.5)
    p = jax.nn.softmax(sc, axis=-1).astype(v.dtype)
    o = jnp.einsum('bhsm,bmhd->bshd', p, v).reshape(b, s, d)
    return o @ wo


def hierarchical_moe(x, w_group, b_group, w_expert, b_expert, w_gate, w_up, w_down):
    b, s, d = x.shape
    xt = x.reshape(-1, d)
    n_tok = xt.shape[0]
    g_prob = jax.nn.softmax((xt @ w_group + b_group).astype(jnp.float32), axis=-1)
    g_w, g_idx = lax.top_k(g_prob, 1)
    e_logits = (xt @ w_expert + b_expert).astype(jnp.float32).reshape(n_tok, MOE_GROUPS, MOE_EXPERTS_PER_GROUP)
    e_logits = e_logits[jnp.arange(n_tok), g_idx[:, 0]]
    top_p, top_i = lax.top_k(jax.nn.softmax(e_logits, axis=-1), MOE_TOP_K)
    top_p = top_p / jnp.sum(top_p, axis=-1, keepdims=True)
    weights = (g_w * top_p).reshape(-1)
    experts = (g_idx * MOE_EXPERTS_PER_GROUP + top_i).reshape(-1)
    tokens = jnp.repeat(jnp.arange(n_tok, dtype=jnp.int32), MOE_TOP_K)
    n_assign = n_tok * MOE_TOP_K
    order = jnp.argsort(experts)
    e_sorted, tok_sorted, w_sorted = experts[order], tokens[order], weights[order]
    counts = jnp.bincount(experts, length=MOE_N_EXPERTS)
    padded = ((counts + MOE_BLOCK - 1) // MOE_BLOCK) * MOE_BLOCK
    start = jnp.cumsum(counts) - counts
    pend = jnp.cumsum(padded)
    pstart = pend - padded
    dest = pstart[e_sorted] + (jnp.arange(n_assign) - start[e_sorted])
    cap = n_assign + MOE_N_EXPERTS * MOE_BLOCK
    n_blocks = cap // MOE_BLOCK
    slot_tok = jnp.full((cap,), n_tok, jnp.int32).at[dest].set(tok_sorted)
    slot_w = jnp.zeros((cap,), jnp.float32).at[dest].set(w_sorted)
    block_expert = jnp.minimum(jnp.searchsorted(pend, jnp.arange(n_blocks) * MOE_BLOCK, side='right'),
                               MOE_N_EXPERTS - 1)
    x_pad = jnp.concatenate([xt, jnp.zeros((1, d), xt.dtype)], axis=0)
    xb = x_pad[slot_tok].reshape(n_blocks, MOE_BLOCK, d)

    def run_block(args):
        xblk, e = args
        hid = jax.nn.silu(xblk @ w_gate[e]) * (xblk @ w_up[e])
        return hid @ w_down[e]

    yb = lax.map(run_block, (xb, block_expert)).reshape(cap, d)
    y = yb * slot_w[:, None].astype(yb.dtype)
    out = jax.ops.segment_sum(y, slot_tok, num_segments=n_tok + 1)[:n_tok]
    return out.reshape(b, s, d)


def setup_inputs(seed: int = 0) -> dict:
    key = jax.random.key(seed)
    ks = jax.random.split(key, 32)
    f32 = jnp.float32
    nrm = lambda k, shape, sc: jax.random.normal(k, shape, f32) * sc
    beta = DEEPNORM_BETA
    col_scale = jnp.concatenate([jnp.full((n,), beta if v else 1.0, f32) for n, v in zip(IN_SPLITS, VALUE_SPLITS)])
    ln_g = lambda k: 1.0 + nrm(k, (DEPTH, D_MODEL), 0.02)
    ln_b = lambda k: nrm(k, (DEPTH, D_MODEL), 0.02)
    return {
        "x": nrm(ks[0], (BATCH, SEQ, D_MODEL), 1.0),
        "mem": nrm(ks[1], (BATCH, N_MEM, D_MODEL), 1.0),
        "w_in": nrm(ks[2], (DEPTH, D_MODEL, IN_COLS), D_MODEL ** -0.5) * col_scale,
        "cmp_pe_k": nrm(ks[3], (DEPTH, CMP_BLOCK, NSA_HEAD_DIM), 0.1),
        "cmp_pe_v": nrm(ks[4], (DEPTH, CMP_BLOCK, NSA_HEAD_DIM), 0.1),
        "cmp_w1_k": nrm(ks[5], (DEPTH, CMP_BLOCK * NSA_HEAD_DIM, CMP_HIDDEN), (CMP_BLOCK * NSA_HEAD_DIM) ** -0.5),
        "cmp_w2_k": nrm(ks[6], (DEPTH, CMP_HIDDEN, NSA_HEAD_DIM), CMP_HIDDEN ** -0.5),
        "cmp_w1_v": nrm(ks[7], (DEPTH, CMP_BLOCK * NSA_HEAD_DIM, CMP_HIDDEN), (CMP_BLOCK * NSA_HEAD_DIM) ** -0.5),
        "cmp_w2_v": nrm(ks[8], (DEPTH, CMP_HIDDEN, NSA_HEAD_DIM), CMP_HIDDEN ** -0.5),
        "nsa_norm_g": 1.0 + nrm(ks[9], (DEPTH, NSA_WIDTH), 0.02),
        "hg_lb_logits": nrm(ks[10], (DEPTH + 1, HG_HEADS * HG_KEY_DIM), 0.5),
        "hg_norm_g": 1.0 + nrm(ks[11], (DEPTH, HG_VAL_DIM), 0.02),
        "w_out": nrm(ks[12], (DEPTH, D_MODEL, D_MODEL), D_MODEL ** -0.5 * beta),
        "ln1_g": ln_g(ks[13]),
        "ln1_b": ln_b(ks[14]),
        "xa_wq": nrm(ks[15], (DEPTH, D_MODEL, D_MODEL), D_MODEL ** -0.5),
        "xa_wk": nrm(ks[16], (DEPTH, D_MODEL, D_MODEL), D_MODEL ** -0.5),
        "xa_wv": nrm(ks[17], (DEPTH, D_MODEL, D_MODEL), D_MODEL ** -0.5 * beta),
        "xa_wo": nrm(ks[18], (DEPTH, D_MODEL, D_MODEL), D_MODEL ** -0.5 * beta),
        "ln2_g": ln_g(ks[19]),
        "ln2_b": ln_b(ks[20]),
        "moe_w_group": nrm(ks[21], (DEPTH, D_MODEL, MOE_GROUPS), D_MODEL ** -0.5),
        "moe_b_group": nrm(ks[22], (DEPTH, MOE_GROUPS), 0.01),
        "moe_w_expert": nrm(ks[23], (DEPTH, D_MODEL, MOE_N_EXPERTS), D_MODEL ** -0.5),
        "moe_b_expert": nrm(ks[24], (DEPTH, MOE_N_EXPERTS), 0.01),
        "moe_w_gate": nrm(ks[25], (DEPTH, MOE_N_EXPERTS, D_MODEL, MOE_D_FF), D_MODEL ** -0.5),
        "moe_w_up": nrm(ks[26], (DEPTH, MOE_N_EXPERTS, D_MODEL, MOE_D_FF), D_MODEL ** -0.5 * beta),
        "moe_w_down": nrm(ks[27], (DEPTH, MOE_N_EXPERTS, MOE_D_FF, D_MODEL), MOE_D_FF ** -0.5 * beta),
        "ln3_g": ln_g(ks[28]),
        "ln3_b": ln_b(ks[29]),
    }


def reference(x, mem, w_in, cmp_pe_k, cmp_pe_v, cmp_w1_k, cmp_w2_k, cmp_w1_v, cmp_w2_v, nsa_norm_g,
              hg_lb_logits, hg_norm_g, w_out, ln1_g, ln1_b, xa_wq, xa_wk, xa_wv, xa_wo, ln2_g, ln2_b,
              moe_w_group, moe_b_group, moe_w_expert, moe_b_expert, moe_w_gate, moe_w_up, moe_w_down,
              ln3_g, ln3_b):
    b, s, _ = x.shape
    lb_all = jnp.cumsum(jax.nn.softmax(hg_lb_logits.astype(jnp.float32), axis=0), axis=0)
    for l in range(DEPTH):
        h = x @ w_in[l]
        (nq, nkc, nvc, nks, nvs, nkw, nvw, ngate, hq, hf, hi, hgate) = split_cols(h)
        kv = lambda a: a.reshape(b, s, NSA_KV_GROUPS, NSA_HEAD_DIM)
        kc = nsa_compress(kv(nkc), cmp_pe_k[l], cmp_w1_k[l], cmp_w2_k[l])
        vc = nsa_compress(kv(nvc), cmp_pe_v[l], cmp_w1_v[l], cmp_w2_v[l])
        tr = lambda a: jnp.transpose(kv(a), (0, 2, 1, 3))
        q_nsa = jnp.transpose(nq.reshape(b, s, NSA_HEADS, NSA_HEAD_DIM), (0, 2, 1, 3))
        gates = jnp.transpose(jax.nn.sigmoid(ngate.reshape(b, s, NSA_HEADS, 3)), (0, 2, 1, 3))
        o_nsa = nsa_attention(q_nsa, kc, vc, tr(nks), tr(nvs), tr(nkw), tr(nvw), gates)
        o_nsa = rms_norm(o_nsa, nsa_norm_g[l])
        o_hg = hgrn2_recurrence(hq.reshape(b, s, HG_HEADS, HG_KEY_DIM),
                                hf.reshape(b, s, HG_HEADS, HG_KEY_DIM),
                                hi.reshape(b, s, HG_HEADS, HG_VAL_DIM), lb_all[l])
        o_hg = rms_norm(o_hg, hg_norm_g[l]) * jax.nn.silu(
            hgate.reshape(b, s, HG_HEADS, HG_VAL_DIM).astype(jnp.float32))
        o_hg = o_hg.reshape(b, s, HG_WIDTH)
        mix = jnp.concatenate([o_nsa.astype(x.dtype), o_hg.astype(x.dtype)], axis=-1) @ w_out[l]
        x = layer_norm(DEEPNORM_ALPHA * x + mix, ln1_g[l], ln1_b[l])
        xa = memory_cross_attention(x, mem, xa_wq[l], xa_wk[l], xa_wv[l], xa_wo[l])
        x = layer_norm(DEEPNORM_ALPHA * x + xa, ln2_g[l], ln2_b[l])
        ff = hierarchical_moe(x, moe_w_group[l], moe_b_group[l], moe_w_expert[l], moe_b_expert[l],
                              moe_w_gate[l], moe_w_up[l], moe_w_down[l])
        x = layer_norm(DEEPNORM_ALPHA * x + ff, ln3_g[l], ln3_b[l])
    return x
```

```python
import numpy as np
import ml_dtypes
import concourse.bass as bass
import concourse.mybir as mybir
from concourse.bass_utils import run_bass_kernel_spmd

F32 = mybir.dt.float32
BF16 = mybir.dt.bfloat16
AF = mybir.ActivationFunctionType
ALU = mybir.AluOpType

D = 1024
NEG = -30000.0
ALPHA = 2.0 ** 0.25
Q0, KC0, VC0, KS0, VS0, KW0, VW0, G0, HQ0, HF0, HI0, HG0, WEND = (
    0, 512, 640, 768, 896, 1024, 1152, 1280, 1304, 1816, 2328, 2840, 3352)
WB = 768
oKS, oVS, oKW, oVW, oG, oHQ, oHF, oHI, oHG = (KS0 - WB, VS0 - WB, KW0 - WB, VW0 - WB, G0 - WB,
                                              HQ0 - WB, HF0 - WB, HI0 - WB, HG0 - WB)
WN = WEND - WB


class Tok:
    __slots__ = ("name", "w", "rs", "excl")

    def __init__(self, name="", excl=False):
        self.name = name
        self.w = None
        self.rs = []
        self.excl = excl


class Op:
    __slots__ = ("eng", "fn", "deps", "dma", "rank", "sem", "val", "sig", "prev_dma")

    def __init__(self, eng, fn, dma):
        self.eng = eng
        self.fn = fn
        self.dma = dma
        self.deps = []
        self.rank = 0
        self.sem = None
        self.val = 0
        self.sig = False
        self.prev_dma = None


class Prog:
    ENGS = ("pe", "act", "dve", "pool", "sp")

    def __init__(self, nc, n_dma_sems=32, n_pool_sems=16):
        self.nc = nc
        self.ops = {e: [] for e in self.ENGS}
        self.n_dma_sems = n_dma_sems
        self.dma_last = [None] * n_dma_sems
        self.dma_cnt = [0] * n_dma_sems
        self.dma_rr = 0
        self.dma_rr2 = 0
        self.n_pool_sems = n_pool_sems

    def op(self, eng, fn, reads=(), writes=(), dma=False):
        o = Op(eng, fn, dma)
        ex = [t for t in reads if t.excl and t not in writes]
        if ex:
            writes = tuple(writes) + tuple(ex)
        deps = []
        for t in reads:
            if t.w is not None:
                deps.append((t.w, True))
        for t in writes:
            if t.w is not None:
                deps.append((t.w, False))
            for r in t.rs:
                deps.append((r, False))
        fl = []
        seen = {}
        for d, raw in deps:
            if d is o:
                continue
            if id(d) in seen:
                if raw:
                    seen[id(d)][1] = True
                continue
            ent = [d, raw]
            seen[id(d)] = ent
            fl.append(ent)
        out = []
        for d, raw in fl:
            if (not dma) and (not d.dma) and d.eng == eng:
                if eng == "pe":
                    continue
            out.append(d)
        if dma:
            if eng == "pool":
                k = self.n_dma_sems - self.n_pool_sems + self.dma_rr2
                self.dma_rr2 = (self.dma_rr2 + 1) % self.n_pool_sems
            else:
                k = self.dma_rr
                self.dma_rr = (k + 1) % (self.n_dma_sems - self.n_pool_sems)
            o.sem = k
            self.dma_cnt[k] += 1
            o.val = 16 * self.dma_cnt[k]
            o.prev_dma = self.dma_last[k]
            self.dma_last[k] = o
            o.sig = True
        for d in out:
            d.sig = True
        o.deps = out
        for t in reads:
            if not dma:
                t.rs = [r for r in t.rs if r.dma or r.eng != eng]
            t.rs.append(o)
        for t in writes:
            t.w = o
            t.rs = []
        self.ops[eng].append(o)
        return o

    def barrier(self):
        lasts = []
        for e in self.ENGS:
            for o in reversed(self.ops[e]):
                if not o.dma and o.fn is not None:
                    lasts.append(o)
                    break
        lasts += [d for d in self.dma_last if d is not None]
        for o in lasts:
            o.sig = True
        for e in self.ENGS:
            b = Op(e, None, False)
            b.deps = [d for d in lasts if d.dma or d.eng != e]
            self.ops[e].append(b)

    def dma(self, out, in_, reads=(), writes=(), eng="sp", **kw):
        def fn(e):
            return e.dma_start(out=out, in_=in_, **kw)
        return self.op(eng, fn, reads, writes, dma=True)

    def emit(self):
        nc = self.nc
        from contextlib import ExitStack
        with ExitStack() as es:
            esem = {}
            for e in ("pe", "act", "dve", "pool"):
                esem[e] = es.enter_context(nc.semaphore("s_" + e))
            dsem = [es.enter_context(nc.semaphore("s_dma%d" % i)) for i in range(self.n_dma_sems)]
            for e in ("pe", "act", "dve", "pool"):
                r = 0
                for o in self.ops[e]:
                    if o.dma or o.fn is None:
                        continue
                    if o.sig:
                        r += 1
                        o.rank = r
            print("ops per engine:", {e: len(self.ops[e]) for e in self.ENGS},
                  "signalling:", {e: sum(1 for o in self.ops[e] if o.sig and not o.dma and o.fn is not None) for e in self.ENGS},
                  "dma max val:", max(self.dma_cnt) * 16)
            block = es.enter_context(nc.Block())

            def run(ename):
                def body(eh):
                    wm_e = {}
                    wm_d = {}

                    def wait_for(d):
                        if d.dma:
                            if wm_d.get(d.sem, 0) >= d.val:
                                return
                            wm_d[d.sem] = d.val
                            eh.wait_ge(dsem[d.sem], d.val)
                        else:
                            if wm_e.get(d.eng, 0) >= d.rank:
                                return
                            wm_e[d.eng] = d.rank
                            eh.wait_ge(esem[d.eng], d.rank)

                    for o in self.ops[ename]:
                        for d in o.deps:
                            wait_for(d)
                        if o.dma and o.prev_dma is not None:
                            wait_for(o.prev_dma)
                        if o.fn is None:
                            continue
                        ins = o.fn(eh)
                        if o.dma:
                            ins.then_inc(dsem[o.sem], 16)
                        elif o.sig:
                            ins.then_inc(esem[ename], 1)
                    if ename == "sp":
                        for k in range(self.n_dma_sems):
                            if self.dma_last[k] is not None:
                                wait_for(self.dma_last[k])
                return body

            block.tensor(run("pe"))
            block.scalar(run("act"))
            block.vector(run("dve"))
            block.gpsimd(run("pool"))
            block.sync(run("sp"))


class SB:
    def __init__(self, nc, base=16512):
        self.nc = nc
        self.cur = base
        self.n = 0
        self.hi = base

    def t(self, shape, dt, name=None):
        sz = 1
        for s in shape[1:]:
            sz *= s
        sz *= 2 if dt == BF16 else 4
        sz = (sz + 31) // 32 * 32
        self.n += 1
        h = self.nc.alloc_sbuf_tensor_at("%s_%d_%d" % (name or "t", self.cur, self.n), list(shape), dt,
                                          offset=self.cur)
        self.cur += sz
        self.hi = max(self.hi, self.cur)
        assert self.cur <= 229376, ("SBUF overflow", self.cur)
        return h


def build(S, debug=None):
    NSUP = S // 512
    NT = S // 128
    NOWN = NSUP
    NCT = (S // 16 + 127) // 128
    debug = debug or {}
    nc = bass.Bass("TRN2", target_bir_lowering=False)
    P = Prog(nc)

    def din(name, shape, dt=F32):
        return nc.dram_tensor(name, list(shape), dt, kind="ExternalInput").ap()

    xfull = din("xfull", [S, D])
    xown = din("xown", [NOWN * 128, D])
    mem = din("mem", [256, D])
    w_in = din("w_in", [D, WEND])
    w1k = din("w1k", [2048, 256])
    w1v = din("w1v", [2048, 256])
    w2k = din("w2k", [256, 64])
    w2v = din("w2v", [256, 64])
    peT = din("peT", [128, 32])
    lbl_bc = din("lbl_bc", [128, 2 * 512])
    lbl_km = din("lbl_km", [128, 8])
    nsag_bc = din("nsag_bc", [128, 512])
    hgg_bc = din("hgg_bc", [128, 512])
    c_f32 = din("c_f32", [128, 128 * 3 + 2])
    c_msel = din("c_msel", [128, 33])
    c_ex32 = din("c_ex32", [128, 64 * 128])
    c_seg = din("c_seg", [128, 512])
    c_moe = din("c_moe", [128, 128 + 12 + 192])
    m_core = din("m_core", [128, 16 * 128])
    v_core = din("v_core", [128, 21])
    w_out = din("w_out", [D, D])
    wq = din("xa_wq", [D, D])
    wk = din("xa_wk", [D, D])
    wv = din("xa_wv", [D, D])
    wo = din("xa_wo", [D, D])
    ln_bc = din("ln_bc", [128, 6 * D])
    mo_wr = din("mo_wr", [D, 36])
    mo_br = din("mo_br", [128, 36])
    mo_wgu = din("mo_wgu", [32 * 4 * 128, 2 * 1024])
    mo_wd2 = din("mo_wd2", [32 * 2 * 128, 2 * 1024])
    out = nc.dram_tensor("out", [NOWN * 128, D], F32, kind="ExternalOutput").ap()
    mixT_d = nc.dram_tensor("mixT_d", [NOWN, 128, 8 * 128], BF16, kind="Internal").ap()
    x2_d = nc.dram_tensor("x2_d", [NOWN * 128, D], F32, kind="Internal").ap()
    dbg = {}
    for k, shp in debug.items():
        if not isinstance(shp, (list, tuple)):
            continue
        dbg[k] = nc.dram_tensor("dbg_" + k, list(shp), F32, kind="ExternalOutput").ap()

    PS = [nc.alloc_psum_tensor("ps%d" % i, [128, 512], F32) for i in range(8)]
    PSB = [nc.alloc_psum_tensor("psb%d" % i, [128, 1024], BF16) for i in range(0)]
    pst = [Tok("ps%d" % i, excl=True) for i in range(8)]

    def act(out_, in_, func, reads, writes, **kw):
        return P.op("act", lambda e: e.activation(out=out_, in_=in_, func=func, **kw), reads, writes)

    def mm(out_, lhsT, rhs, reads, writes, start=True, stop=True, **kw):
        return P.op("pe", lambda e: e.matmul(out_, lhsT, rhs, start=start, stop=stop, **kw), reads, writes)

    def tr(out_, in_, ident, reads, writes):
        return P.op("pe", lambda e: e.transpose(out_, in_, ident), reads, writes)

    def V(eng, name, reads, writes, *a, **kw):
        return P.op(eng, lambda e: getattr(e, name)(*a, **kw), reads, writes)

    sb = SB(nc)
    cf = sb.t([128, 386], F32, "cf"); t_cf = Tok()
    P.dma(cf[:, :], c_f32, (), (t_cf,))
    identf = cf[:, 0:128]; msuf = cf[:, 128:256]; chind = cf[:, 384:386]
    cb = sb.t([128, 128 * 2], BF16, "cb"); t_cb = Tok()
    P.dma(cb[:, 0:128], c_f32[:, 0:128], (), (t_cb,), eng="pool")
    P.dma(cb[:, 128:256], c_f32[:, 256:384], (), (t_cb,), eng="pool")
    identb = cb[:, 0:128]; trib = cb[:, 128:256]
    msel = sb.t([128, 33], BF16, "msel"); t_msel = Tok()
    P.dma(msel[:, :], c_msel, (), (t_msel,), eng="pool")
    ex32 = sb.t([128, 64, 128], BF16, "ex32"); t_ex = Tok()
    P.dma(ex32[:, :, :], c_ex32.rearrange("p (v k) -> p v k", v=64), (), (t_ex,), eng="pool")
    seg = sb.t([128, 512], F32, "seg"); t_seg = Tok()
    P.dma(seg[:, :], c_seg, (), (t_seg,))
    mcore = sb.t([128, 16, 128], BF16, "mcore"); t_mc = Tok()
    P.dma(mcore[:, :, :], m_core.rearrange("p (v k) -> p v k", v=16), (), (t_mc,), eng="pool")
    vcore = sb.t([128, 21], F32, "vcore"); t_vc = Tok()
    P.dma(vcore[:, :], v_core, (), (t_vc,))
    zrow = sb.t([128, 512], BF16, "zrow"); t_z = Tok()
    V("pool", "memset", (), (t_z,), zrow[:, :], 0.0)
    eps6 = sb.t([128, 2], F32, "eps6")
    V("pool", "memset", (), (t_z,), eps6[:, 0:1], 1e-6)
    V("pool", "memset", (), (t_z,), eps6[:, 1:2], 1e-5)
    onesb = sb.t([128, 2], BF16, "onesb")
    V("pool", "memset", (), (t_z,), onesb[:, :], 1.0)

    if debug.get("cut1"):
        P.emit()
        return nc
    W = sb.t([128, 8, WN], BF16, "W"); t_W = Tok()
    w_in_v = w_in.rearrange("(kt p) c -> p kt c", p=128)
    for kt in range(8):
        P.dma(W[:, kt, :], w_in_v[:, kt, WB:WEND], (), (t_W,), eng="pool")
    Wq = sb.t([128, 8, 4, 128], BF16, "Wq")
    for r in range(4):
        for g in range(2):
            h = g * 4 + r
            P.dma(Wq[:, :, r, g * 64:(g + 1) * 64], w_in_v[:, :, h * 64:(h + 1) * 64], (), (t_W,), eng="pool")
    Wcd = sb.t([128, 8, 4, 128], BF16, "Wcd")
    for idx, base in enumerate((KC0, KC0 + 64, VC0, VC0 + 64)):
        for dup in range(2):
            P.dma(Wcd[:, :, idx, dup * 64:(dup + 1) * 64], w_in_v[:, :, base:base + 64], (), (t_W,), eng="pool")
    W1 = sb.t([128, 2, 16, 256], BF16, "W1")
    P.dma(W1[:, 0, :, :], w1k.rearrange("(jp p) h -> p jp h", p=128), (), (t_W,), eng="pool")
    P.dma(W1[:, 1, :, :], w1v.rearrange("(jp p) h -> p jp h", p=128), (), (t_W,), eng="pool")
    W2p = sb.t([128, 2, 2, 128], BF16, "W2p")
    V("pool", "memset", (), (t_W,), W2p[:, :, :, :], 0.0)
    for ht in range(2):
        for g in range(2):
            P.dma(W2p[:, ht, g, g * 64:(g + 1) * 64], w2k[ht * 128:(ht + 1) * 128, :], (), (t_W,), eng="pool")
    W2pv = sb.t([128, 2, 2, 128], BF16, "W2pv")
    V("pool", "memset", (), (t_W,), W2pv[:, :, :, :], 0.0)
    for ht in range(2):
        for g in range(2):
            P.dma(W2pv[:, ht, g, g * 64:(g + 1) * 64], w2v[ht * 128:(ht + 1) * 128, :], (), (t_W,), eng="pool")
    VCT = sb.t([128, NCT * 128], BF16, "VCT")
    pet = sb.t([128, 32], BF16, "pet")
    P.dma(pet[:, :], peT, (), (t_W,), eng="pool")
    nsag = sb.t([128, 512], F32, "nsag"); hgg = sb.t([128, 512], F32, "hgg")
    P.dma(nsag[:, :], nsag_bc, (), (t_W,))
    P.dma(hgg[:, :], hgg_bc, (), (t_W,))
    if debug.get("cut2"):
        P.emit()
        return nc
    hh = sb.t([128, 3, 512], F32, "hh")
    lbt = hh[:, 0:2, :].rearrange("p a b -> p (a b)"); t_lb = Tok()
    P.dma(lbt, lbl_bc, (), (t_lb,))
    lbk = sb.t([128, 8], F32, "lbk")
    P.dma(lbk[:, :], lbl_km, (), (t_lb,))
    lb_bc = sb.t([128, 512], F32, "lb_bc"); oml_bc = sb.t([128, 512], F32, "oml_bc")
    lb_k = sb.t([128, 4], F32, "lb_k"); oml_k = sb.t([128, 4], F32, "oml_k")
    for (src0, src1, lbo, omlo) in ((lbt[:, 0:512], lbt[:, 512:1024], lb_bc[:, :], oml_bc[:, :]),
                                     (lbk[:, 0:4], lbk[:, 4:8], lb_k[:, :], oml_k[:, :])):
        V("dve", "tensor_sub", (t_lb,), (t_lb,), omlo, src1, src0)
        act(omlo, omlo, AF.Exp, (t_lb,), (t_lb,))
        V("dve", "tensor_scalar_add", (t_lb,), (t_lb,), omlo, omlo, 1.0)
        V("dve", "reciprocal", (t_lb,), (t_lb,), lbo, omlo)
        V("dve", "tensor_scalar", (t_lb,), (t_lb,), omlo, lbo, -1.0, 1.0, ALU.mult, ALU.add)

    if debug.get("cut3"):
        P.emit()
        return nc
    biasc = sb.t([128, 4], F32, "biasc"); t_bias = Tok()
    for kv in range(2):
        for ht in range(2):
            for jp in range(16):
                mm(PS[0][:, kv * 2 + ht:kv * 2 + ht + 1], W1[:, kv, jp, ht * 128:(ht + 1) * 128],
                   pet[:, kv * 16 + jp:kv * 16 + jp + 1], (t_W,), (pst[0],), start=(jp == 0), stop=(jp == 15))
    V("dve", "tensor_copy", (pst[0],), (t_bias,), biasc[:, :], PS[0][:, 0:4])

    if debug.get("cut4"):
        P.emit()
        return nc
    ksT_d = nc.dram_tensor("ksT_d", [128, S], BF16, kind="Internal").ap()
    vsA_d = nc.dram_tensor("vsA_d", [128, NT * 130], BF16, kind="Internal").ap()
    kvst = [sb.t([128, 128 + 130], BF16, "kvst") for _ in range(2)]; t_kvst = [Tok(), Tok()]
    NRING = 4
    kring = [sb.t([128, 512 + 4 * 130], BF16, "kring") for _ in range(NRING)]; t_ring = [Tok() for _ in range(NRING)]
    kwR = sb.t([128, 16, 128], BF16, "kwR")
    vwR = sb.t([128, 16, 2, 65], BF16, "vwR")
    KCT = sb.t([128, NCT * 128], BF16, "KCT")
    VCA = sb.t([128, NCT, 2, 65], BF16, "VCA")
    t_ks = [Tok() for _ in range(NT)]
    t_kw = [Tok() for _ in range(16)]
    t_kc = [Tok() for _ in range(NCT)]
    t_init = Tok()
    V("pool", "memset", (), (t_init,), KCT[:, :], 0.0)
    V("pool", "memset", (), (t_init,), VCT[:, :], 0.0)
    V("pool", "memset", (), (t_init,), VCA[:, :, :, :], 0.0)
    for q_ in range(2):
        V("pool", "memset", (), (t_kvst[q_],), kvst[q_][:, 128:258], 1.0)
    V("pool", "memset", (), (t_init,), vwR[:, :, :, 64:65], 1.0)
    for ct in range(NCT):
        V("pool", "memset", (t_init,), (t_kc[ct],), VCA[:, ct, :, 64:65], 1.0)
    for j in range(16):
        t_kw[j].w = t_init.w
    KKW = 16 + 512 + 16
    KK = sb.t([128, 2, 4, KKW], BF16, "KK")
    t_kk = [Tok(), Tok()]
    V("pool", "memset", (), (t_kk[0], t_kk[1]), KK[:, :, :, :], 0.0)

    Sst = sb.t([128, 512], F32, "Sst"); t_S = Tok()
    V("pool", "memset", (), (t_S,), Sst[:, :], 0.0)
    Sown = sb.t([128, 2, 2, 512], BF16, "Sown")
    t_so = [Tok(), Tok()]

    if debug.get("cut5"):
        P.emit()
        return nc
    NXB = 2
    xs = [sb.t([128, D], F32, "xs") for _ in range(NXB)]; t_xs = [Tok() for _ in range(NXB)]
    xT = [sb.t([128, 8, 128], BF16, "xT") for _ in range(2)]; t_xT = [Tok(), Tok()]
    xTo = sb.t([128, 8, 128], BF16, "xTo"); t_xTo = Tok()
    vsb = sb.t([128, 512], BF16, "vsb"); t_vsb = Tok()
    h1 = hh[:, 0, :]; h2 = hh[:, 1, :]; h3 = hh[:, 2, :]
    t_h1, t_h2, t_h3 = Tok(), Tok(), Tok()
    t_h1.w = t_lb.w; t_h2.w = t_lb.w; t_h1.rs = t_lb.rs; t_h2.rs = t_lb.rs
    kkb = sb.t([128, 2, 512], BF16, "kkb"); t_kkb = Tok()
    V("pool", "memset", (), (t_kkb,), kkb[:, :, :], 0.0)
    dec = sb.t([128, 8], F32, "dec"); t_dec = Tok()
    blt = sb.t([128, 512], BF16, "blt"); t_bl = Tok()

    def load_xT(src_ap, k, XT, tXT, banks=(0, 1)):
        b = k % NXB
        P.dma(xs[b][:, :], src_ap, (), (t_xs[b],))
        for half in range(2):
            bk = banks[half]
            bank = PS[bk]
            for q in range(4):
                kt = half * 4 + q
                tr(bank[:, q * 128:(q + 1) * 128], xs[b][:, kt * 128:(kt + 1) * 128], identf,
                   (t_xs[b], t_cf), (pst[bk],))
            if half == 0:
                act(XT[:, 0:4, :], bank[:, :].rearrange("p (a b) -> p a b", a=4), AF.Copy,
                    (pst[bk],), (tXT,))
            else:
                V("dve", "tensor_copy", (pst[bk],), (tXT,), XT[:, 4:8, :],
                  bank[:, :].rearrange("p (a b) -> p a b", a=4))

    def full_tile(j, kidx):
        p = kidx % 2
        load_xT(xfull[j * 128:(j + 1) * 128, :], kidx, xT[p], t_xT[p], banks=(2, 4))
        X = xT[p]; tX = t_xT[p]
        yield
        sup = j // 4
        par = sup % 2
        for n_, off in enumerate((oKS, oKW)):
            for kt in range(8):
                mm(PS[5][:, n_ * 128:(n_ + 1) * 128], W[:, kt, off:off + 128], X[:, kt, :],
                   (t_W, tX), (pst[5],), start=(kt == 0), stop=(kt == 7))
        slot = j % 16
        sq = j % 2
        act(kvst[sq][:, 0:128], PS[5][:, 0:128], AF.Copy, (pst[5],), (t_kvst[sq],))
        V("dve", "tensor_copy", (pst[5],), (t_kw[slot],), kwR[:, slot, :], PS[5][:, 128:256])
        yield
        for idx in range(4):
            for kt in range(8):
                mm(PS[2][:, idx * 128:(idx + 1) * 128], Wcd[:, kt, idx, :], X[:, kt, :],
                   (t_W, tX), (pst[2],), start=(kt == 0), stop=(kt == 7))
        lo = 16 + (j % 4) * 128
        act(KK[0:64, par, :, lo:lo + 128], PS[2][0:64, :].rearrange("p (a b) -> p a b", a=4), AF.Copy,
            (pst[2],), (t_kk[par],))
        V("dve", "tensor_copy", (pst[2],), (t_kk[par],), KK[64:128, par, :, lo - 1:lo + 127],
          PS[2][64:128, :].rearrange("p (a b) -> p a b", a=4))
        if j % 4 == 0 and j > 0:
            V("pool", "tensor_copy", (t_kk[par],), (t_kk[1 - par],), KK[0:64, 1 - par, :, 528:544],
              KK[0:64, par, :, 16:32])
            V("pool", "tensor_copy", (t_kk[par],), (t_kk[1 - par],), KK[64:128, 1 - par, :, 527:544],
              KK[64:128, par, :, 15:32])
        yield
        for n_, off in enumerate((oVS, oVW)):
            for kt in range(8):
                mm(PS[4][:, n_ * 128:(n_ + 1) * 128], X[:, kt, :], W[:, kt, off:off + 128],
                   (t_W, tX), (pst[4],), start=(kt == 0), stop=(kt == 7))
        act(kvst[sq][:, 128:258].rearrange("p (g d) -> p g d", g=2)[:, :, 0:64],
            PS[4][:, 0:128].rearrange("p (g d) -> p g d", g=2), AF.Copy, (pst[4],), (t_kvst[sq],))
        P.dma(ksT_d[:, j * 128:(j + 1) * 128], kvst[sq][:, 0:128], (t_kvst[sq],), (t_ks[j],))
        P.dma(vsA_d[:, j * 130:(j + 1) * 130], kvst[sq][:, 128:258], (t_kvst[sq],), (t_ks[j],))
        V("dve", "tensor_copy", (pst[4],), (t_kw[slot],), vwR[:, slot, :, 0:64],
          PS[4][:, 128:256].rearrange("p (g d) -> p g d", g=2))
        yield
        for bank, off in ((5, oHF), (2, oHI)):
            for kt in range(8):
                mm(PS[bank][:, :], X[:, kt, :], W[:, kt, off:off + 512], (t_W, tX), (pst[bank],),
                   start=(kt == 0), stop=(kt == 7))
            yield
        act(vsb[:, :], PS[2][:, :], AF.Copy, (pst[2],), (t_vsb,))
        act(h1[:, :], PS[5][:, :], AF.Exp, (pst[5],), (t_h1,), scale=-1.0)
        V("dve", "tensor_scalar_add", (t_h1,), (t_h1,), h1[:, :], h1[:, :], 1.0); V("dve", "reciprocal", (t_h1,), (t_h1,), h1[:, :], h1[:, :])
        V("dve", "tensor_mul", (t_h1, t_lb), (t_h1,), h1[:, :], h1[:, :], oml_bc[:, :])
        V("dve", "tensor_add", (t_h1, t_lb), (t_h1,), h1[:, :], h1[:, :], lb_bc[:, :])
        act(h2[:, :], h1[:, :], AF.Ln, (t_h1,), (t_h2,))
        V("dve", "tensor_scalar", (t_h1,), (t_h1,), h1[:, :], h1[:, :], -1.0, 1.0, ALU.mult, ALU.add)
        yield
        mm(PS[4][:, :], msuf, h2[:, :], (t_cf, t_h2), (pst[4],))
        act(h3[:, :], PS[4][:, :], AF.Exp, (pst[4],), (t_h3,))
        for c in range(2):
            V("dve", "tensor_mul", (t_h1, t_h3), (t_kkb,), kkb[64 * c:64 * c + 64, c, :], h1[64 * c:64 * c + 64, :],
              h3[64 * c:64 * c + 64, :])
        for h in range(4):
            mm(PS[5][:, 256 + 2 * h:256 + 2 * h + 2], h2[:, h * 128:(h + 1) * 128], chind, (t_h2, t_cf),
               (pst[5],))
        act(dec[:, :], PS[5][:, 256:264], AF.Exp, (pst[5],), (t_dec,))
        yield
        for c in range(2):
            jj = j % 4
            if jj == 0:
                V("dve", "tensor_scalar_mul", (t_S, t_vc), (t_so[par],), Sown[:, par, c, :], Sst[:, :],
                  vcore[:, 17:18])
            else:
                V("dve", "scalar_tensor_tensor", (t_S, t_vc, t_so[par]), (t_so[par],), Sown[:, par, c, :],
                  Sst[:, :], vcore[:, 17 + jj:18 + jj], Sown[:, par, c, :], ALU.mult, ALU.add)
            for h in range(4):
                mm(PS[4][:, h * 128:(h + 1) * 128], kkb[:, c, h * 128:(h + 1) * 128],
                   vsb[:, h * 128:(h + 1) * 128], (t_kkb, t_vsb), (pst[4],))
            for h in range(4):
                V("dve", "scalar_tensor_tensor", (t_S, t_dec, pst[4]), (t_S,), Sst[:, h * 128:(h + 1) * 128],
                  Sst[:, h * 128:(h + 1) * 128], dec[:, 2 * h + c:2 * h + c + 1],
                  PS[4][:, h * 128:(h + 1) * 128], ALU.mult, ALU.add)
            yield

    u_sb = sb.t([128, 8, 32], F32, "u_sb"); t_u = Tok()
    t_vct = Tok()
    PSb1 = PS[1][:, :].bitcast(BF16)
    u2 = sb.t([128, 256], F32, "u2"); t_u2 = Tok()
    gl = sb.t([128, 8, 32], BF16, "gl"); t_gl = Tok()

    def compress(i):
        par = i % 2
        for kv in range(2):
            for g in range(2):
                idx = kv * 2 + g
                for ht in range(2):
                    col = (idx * 2 + ht) * 32
                    for jp in range(16):
                        rhs = KK[:, par, idx, 16 + 2 * jp:16 + 2 * jp + 497:16]
                        mm(PS[0][:, col:col + 32], W1[:, kv, jp, ht * 128:(ht + 1) * 128], rhs,
                           (t_W, t_kk[par]), (pst[0],), start=(jp == 0), stop=(jp == 15))
        for kv in range(2):
            for ht in range(2):
                for g in range(2):
                    s_ = ((kv * 2 + g) * 2 + ht)
                    act(u_sb[:, s_, :], PS[0][:, s_ * 32:(s_ + 1) * 32], AF.Identity, (pst[0], t_bias), (t_u,),
                        bias=biasc[:, kv * 2 + ht:kv * 2 + ht + 1])
        uf = u_sb[:, :, :].rearrange("p a b -> p (a b)")
        V("dve", "tensor_mul", (t_u,), (t_u2,), u2[:, :], uf, uf)
        V("dve", "tensor_scalar", (t_u2,), (t_u2,), u2[:, :], u2[:, :], 0.044715, 1.0, ALU.mult, ALU.add)
        V("dve", "tensor_mul", (t_u2, t_u), (t_u2,), u2[:, :], u2[:, :], uf)
        act(u2[:, :], u2[:, :], AF.Exp, (t_u2,), (t_u2,), scale=-1.5957691216057308)
        V("dve", "tensor_scalar_add", (t_u2,), (t_u2,), u2[:, :], u2[:, :], 1.0); V("dve", "reciprocal", (t_u2,), (t_u2,), u2[:, :], u2[:, :])
        V("dve", "tensor_mul", (t_u2, t_u), (t_gl,), gl[:, :, :].rearrange("p a b -> p (a b)"), u2[:, :], uf)
        ct = i // 4
        q = i % 4
        first = True
        for g in range(2):
            for ht in range(2):
                s_ = ((0 * 2 + g) * 2 + ht)
                mm(PS[1][:, 0:32], W2p[:, ht, g, :], gl[:, s_, :], (t_W, t_gl), (pst[1],), start=first,
                   stop=(g == 1 and ht == 1))
                first = False
        act(KCT[:, i * 32:(i + 1) * 32], PS[1][:, 0:32], AF.Copy, (pst[1],), (t_kc[ct],))
        first = True
        for g in range(2):
            for ht in range(2):
                s_ = ((1 * 2 + g) * 2 + ht)
                mm(PS[1][:, 64:96], W2pv[:, ht, g, :], gl[:, s_, :], (t_W, t_gl), (pst[1],), start=first,
                   stop=(g == 1 and ht == 1))
                first = False
        V("dve", "tensor_copy", (pst[1],), (t_vct,), VCT[:, i * 32:(i + 1) * 32], PS[1][:, 64:96])
        tr(PSb1[:, 256:384], VCT[:, ct * 128:(ct + 1) * 128], identb, (t_vct, t_cb), (pst[1],))
        V("dve", "tensor_copy", (pst[1],), (t_kc[ct],), VCA[:, ct, :, 0:64],
          PSb1[:, 256:384].rearrange("p (g d) -> p g d", g=2))

    qT = sb.t([128, 2, 4, 128], BF16, "qT"); t_qT = Tok()
    V("pool", "memset", (), (t_qT,), qT[:, :, :, :], 0.0)
    gts = sb.t([128, 24], F32, "gts"); t_g = Tok()
    Pt = [sb.t([128, 512], BF16, "Pt") for _ in range(4)]; t_Pt = [Tok() for _ in range(4)]
    sc = sb.t([128, 256], F32, "sc"); sc2 = sb.t([128, 256], F32, "sc2"); t_sc = Tok()
    m8 = sb.t([128, 16], F32, "m8")
    nsel = sb.t([128, 256], BF16, "nsel"); t_nsel = Tok()
    V("pool", "memset", (), (t_nsel,), nsel[:, :], 0.0)
    nselT = sb.t([128, 2, 2, 4, 128], BF16, "nselT"); t_nselT = [Tok(), Tok()]
    V("pool", "memset", (), (t_nselT[0], t_nselT[1]), nselT[:, :, :, :, :], 0.0)
    rden = sb.t([128, 24], F32, "rden"); t_rd = Tok()
    grd = sb.t([128, 24], F32, "grd"); t_grd = Tok()
    onsa = sb.t([128, 512], F32, "onsa"); t_on = Tok()
    mixn = sb.t([128, 1024], BF16, "mixn"); t_mixn = Tok()
    mixT = [sb.t([128, 8, 128], BF16, "mixT") for _ in range(1)] * 2; t_mixT = [Tok()] * 2
    st6 = sb.t([128, 8, 6], F32, "st6"); mv = sb.t([128, 8, 2], F32, "mv"); t_st = Tok()
    g1 = sb.t([128, 512], F32, "g1"); g2 = sb.t([128, 512], F32, "g2"); t_g1, t_g2 = Tok(), Tok()
    qdT = sb.t([128, 512], BF16, "qdT"); kdT = sb.t([128, 512], BF16, "kdT"); qsT = sb.t([128, 2, 512], BF16, "qsT")
    t_qd, t_kd, t_qs = Tok(), Tok(), Tok()
    V("pool", "memset", (), (t_qs,), qsT[:, :, :], 0.0)
    attm = sb.t([128, 512], BF16, "attm"); t_attm = Tok()
    vob = sb.t([128, 512], BF16, "vob"); t_vob = Tok()
    sgt = sb.t([128, 512], F32, "sgt"); t_sgt = Tok()
    print("phase A SBUF bytes/partition:", sb.cur)
    pcnt = [0]
    rcnt = [0]
    ptc = [0]

    def own_tile(i, kidx, bg=None):
        load_xT(xown[i * 128:(i + 1) * 128, :], kidx, xTo, t_xTo)
        X = xTo; tX = t_xTo
        par = i % 2
        for r in range(4):
            for kt in range(8):
                mm(PS[2][:, r * 128:(r + 1) * 128], Wq[:, kt, r, :], X[:, kt, :], (t_W, tX), (pst[2],),
                   start=(kt == 0), stop=(kt == 7))
        act(qT[0:64, 0, :, :], PS[2][0:64, :].rearrange("p (a b) -> p a b", a=4), AF.Copy, (pst[2],), (t_qT,), scale=0.125)
        act(qT[64:128, 1, :, :], PS[2][64:128, :].rearrange("p (a b) -> p a b", a=4), AF.Copy, (pst[2],), (t_qT,),
            scale=0.125)
        for kt in range(8):
            mm(PS[3][:, 0:24], X[:, kt, :], W[:, kt, oG:oG + 24], (t_W, tX), (pst[3],), start=(kt == 0),
               stop=(kt == 7))
        act(gts[:, :], PS[3][:, 0:24], AF.Exp, (pst[3],), (t_g,), scale=-1.0)
        V("dve", "tensor_scalar_add", (t_g,), (t_g,), gts[:, :], gts[:, :], 1.0); V("dve", "reciprocal", (t_g,), (t_g,), gts[:, :], gts[:, :])

        def zinit(bank, lo, hi):
            mm(PS[bank][:, lo:hi], zrow[:, 0:128], zrow[:, 0:hi - lo], (t_z,), (pst[bank],), start=True, stop=False,
               skip_group_check=True)

        def s_stage(it):
            g = it["g"]
            masks = it["masks"]
            banks = it.get("sbanks", (0, 1))
            sbk = banks[pcnt[0] % len(banks)]
            pcnt[0] += 1
            sps = PS[sbk]
            allwide = all(m[3] for m in masks)
            if allwide:
                mm(sps[:, :], it["kT"], qT[:, g, :, :].rearrange("p a b -> p (a b)"),
                   tuple(it["rds"]) + (t_qT,), (pst[sbk],), start=True, stop=(len(masks) == 0))
                for mi, (ml, mr, mrd, wide) in enumerate(masks):
                    mm(sps[:, :], ml, mr, tuple(mrd), (pst[sbk],), start=False, stop=(mi == len(masks) - 1))
            else:
                for r in range(4):
                    mm(sps[:, r * 128:(r + 1) * 128], it["kT"], qT[:, g, r, :],
                       tuple(it["rds"]) + (t_qT,), (pst[sbk],), start=True, stop=False)
                    for mi, (ml, mr, mrd, wide) in enumerate(masks):
                        mr_ = mr[:, r * 128:(r + 1) * 128] if wide else mr
                        mm(sps[:, r * 128:(r + 1) * 128], ml, mr_, tuple(mrd), (pst[sbk],), start=False,
                           stop=(mi == len(masks) - 1))
            pb = ptc[0] % 4
            ptc[0] += 1
            act(Pt[pb][:, :], sps[:, :], AF.Exp, (pst[sbk],), (t_Pt[pb],))
            return pb

        def pv_stage(it, pb):
            acc = PS[it["acc"]]
            pt_ap = Pt[pb][:, :]; tp = t_Pt[pb]
            for r in range(4):
                mm(acc[:, r * 65:(r + 1) * 65], pt_ap[:, r * 128:(r + 1) * 128], it["vA"], (tp,) + tuple(it["rds"]),
                   (pst[it["acc"]],), start=False, stop=it["last"], skip_group_check=True)
            if it.get("imp_ct") is not None:
                ct = it["imp_ct"]
                wcols = min(33, 256 - 32 * ct)
                for r in range(4):
                    bank = 3 + r // 2
                    base = (r % 2) * 256 + 32 * ct
                    mm(PS[bank][:, base:base + wcols], pt_ap[:, r * 128:(r + 1) * 128], msel[:, 0:wcols],
                       (tp, t_msel), (pst[bank],), start=False, stop=it["last"], skip_group_check=True)

        def run_pipeline(items, depth=1, prep=None, bgstep=None):
            pend = []
            for it in items:
                if it.get("pre") is not None:
                    it["pre"]()
                if prep is not None:
                    prep(it)
                pb = s_stage(it)
                pend.append((it, pb))
                if len(pend) > depth:
                    pv_stage(*pend.pop(0))
                if bgstep is not None:
                    bgstep()
            for pr in pend:
                pv_stage(*pr)

        def finish(g, accbank, br):
            acc = PS[accbank]
            for r in range(4):
                c_ = (g * 4 + r) * 3 + br
                V("dve", "tensor_scalar_max", (pst[accbank],), (t_rd,), rden[:, c_:c_ + 1],
                  acc[:, r * 65 + 64:r * 65 + 65], 1e-30)
                V("dve", "reciprocal", (t_rd,), (t_rd,), rden[:, c_:c_ + 1], rden[:, c_:c_ + 1])
            sl = slice(g * 12 + br, g * 12 + 12, 3)
            V("dve", "tensor_mul", (t_rd, t_g), (t_grd,), grd[:, sl], rden[:, sl], gts[:, sl])

        def gate_out(g, accbank, br):
            acc = PS[accbank]
            for r in range(4):
                h = g * 4 + r
                c_ = h * 3 + br
                if br == 0:
                    V("dve", "tensor_scalar_mul", (pst[accbank], t_grd), (t_on,), onsa[:, h * 64:(h + 1) * 64],
                      acc[:, r * 65:r * 65 + 64], grd[:, c_:c_ + 1])
                else:
                    V("dve", "scalar_tensor_tensor", (pst[accbank], t_grd, t_on), (t_on,),
                      onsa[:, h * 64:(h + 1) * 64], acc[:, r * 65:r * 65 + 64], grd[:, c_:c_ + 1],
                      onsa[:, h * 64:(h + 1) * 64], ALU.mult, ALU.add)

        for n_, off in enumerate((oHQ, oHF)):
            for h in range(4):
                for kt in range(8):
                    mm(PS[2 + n_][:, h * 128:(h + 1) * 128], W[:, kt, off + h * 128:off + (h + 1) * 128], X[:, kt, :],
                       (t_W, tX), (pst[2 + n_],), start=(kt == 0), stop=(kt == 7))
        for bank, off in ((4, oHI), (5, oHG)):
            for kt in range(8):
                mm(PS[bank][:, :], X[:, kt, :], W[:, kt, off:off + 512], (t_W, tX), (pst[bank],), start=(kt == 0),
                   stop=(kt == 7))
        act(vob[:, :], PS[4][:, :], AF.Copy, (pst[4],), (t_vob,))
        act(sgt[:, :], PS[5][:, :], AF.Exp, (pst[5],), (t_sgt,), scale=-1.0)
        V("dve", "tensor_scalar_add", (t_sgt,), (t_sgt,), sgt[:, :], sgt[:, :], 1.0); V("dve", "reciprocal", (t_sgt,), (t_sgt,), sgt[:, :], sgt[:, :])
        V("dve", "tensor_mul", (t_sgt, pst[5]), (t_sgt,), sgt[:, :], sgt[:, :], PS[5][:, :])
        V("dve", "tensor_mul", (t_sgt, t_W), (t_sgt,), sgt[:, :], sgt[:, :], hgg[:, :])
        act(g1[:, :], PS[3][:, :], AF.Exp, (pst[3],), (t_g1,), scale=-1.0)
        V("dve", "tensor_scalar_add", (t_g1,), (t_g1,), g1[:, :], g1[:, :], 1.0); V("dve", "reciprocal", (t_g1,), (t_g1,), g1[:, :], g1[:, :])
        for h in range(4):
            V("dve", "tensor_scalar", (t_g1, t_lb), (t_g1,), g1[:, h * 128:(h + 1) * 128], g1[:, h * 128:(h + 1) * 128],
              oml_k[:, h:h + 1], lb_k[:, h:h + 1], ALU.mult, ALU.add)
        act(g2[:, :], g1[:, :], AF.Ln, (t_g1,), (t_g2,))
        V("dve", "tensor_scalar", (t_g1,), (t_g1,), g1[:, :], g1[:, :], -1.0, 1.0, ALU.mult, ALU.add)
        V("dve", "tensor_tensor_scan", (t_seg, t_g2), (t_h1,), h1[:, :], seg[:, :], g2[:, :], 0.0, ALU.mult, ALU.add)
        act(h2[:, :], h1[:, :], AF.Exp, (t_h1,), (t_h2,))
        for c in range(2):
            V("dve", "tensor_mul", (t_h2, pst[2]), (t_qs,),
              qsT[:, c, :].rearrange("p (h t) -> p h t", h=4)[:, :, 64 * c:64 * c + 64],
              h2[:, :].rearrange("p (h t) -> p h t", h=4)[:, :, 64 * c:64 * c + 64],
              PS[2][:, :].rearrange("p (h t) -> p h t", h=4)[:, :, 64 * c:64 * c + 64])
        for hc in range(8):
            V("dve", "tensor_scalar", (t_h1,), (t_h3,), h3[:, hc * 64:(hc + 1) * 64], h1[:, hc * 64:(hc + 1) * 64],
              h1[:, hc * 64 + 31:hc * 64 + 32], None, ALU.subtract)
        act(h2[:, :], h3[:, :], AF.Exp, (t_h3,), (t_h2,))
        V("dve", "tensor_mul", (t_h2, pst[2]), (t_qd,), qdT[:, :], h2[:, :], PS[2][:, :])
        act(g2[:, :], h3[:, :], AF.Exp, (t_h3,), (t_g2,), scale=-1.0)
        V("dve", "tensor_mul", (t_g2, t_g1), (t_kd,), kdT[:, :], g2[:, :], g1[:, :])
        for h in range(4):
            mm(PS[3][:, h * 128:(h + 1) * 128], kdT[:, h * 128:(h + 1) * 128], qdT[:, h * 128:(h + 1) * 128],
               (t_kd, t_qd), (pst[3],))
        V("dve", "tensor_mul", (pst[3], t_cb), (t_attm,), attm[:, :].rearrange("p (a b) -> p a b", a=4),
          PS[3][:, :].rearrange("p (a b) -> p a b", a=4), trib.unsqueeze(1).broadcast_to([128, 4, 128]))
        mm(PS[4][:, :], zrow[:, 0:128], zrow[:, :], (t_z,), (pst[4],), start=True, stop=False, skip_group_check=True)
        for h in range(4):
            hs = slice(h * 128, (h + 1) * 128)
            mm(PS[4][:, hs], attm[:, hs], vob[:, hs], (t_attm, t_vob), (pst[4],), start=False, stop=False,
               skip_group_check=True)
            for c in range(2):
                mm(PS[4][:, hs], qsT[:, c, hs], Sown[:, par, c, hs],
                   (t_qs, t_so[par]), (pst[4],), start=False, stop=True, skip_group_check=True)
        if debug.get("ohg") is not None:
            V("dve", "tensor_copy", (pst[4],), (t_g1,), g1[:, :], PS[4][:, :])
            P.dma(dbg["ohg"][i * 128:(i + 1) * 128, :], g1[:, :], (t_g1,), ())
        for h in range(4):
            V("dve", "bn_stats", (pst[4],), (t_st,), st6[:, 1 + h, :], PS[4][:, h * 128:(h + 1) * 128])
            V("dve", "bn_aggr", (t_st,), (t_st,), mv[:, 2 + h, :], st6[:, 1 + h, :])
            V("dve", "tensor_mul", (t_st,), (t_st,), st6[:, 1 + h, 0:1], mv[:, 2 + h, 0:1], mv[:, 2 + h, 0:1])
            V("dve", "tensor_add", (t_st,), (t_st,), st6[:, 1 + h, 0:1], st6[:, 1 + h, 0:1], mv[:, 2 + h, 1:2])
            act(st6[:, 1 + h, 1:2], st6[:, 1 + h, 0:1], AF.Ln, (t_st,), (t_st,), bias=eps6[:, 0:1])
            act(st6[:, 1 + h, 1:2], st6[:, 1 + h, 1:2], AF.Exp, (t_st,), (t_st,), scale=-0.5)
            V("dve", "scalar_tensor_tensor", (pst[4], t_st, t_sgt), (t_mixn,), mixn[:, 512 + h * 128:512 + (h + 1) * 128],
              PS[4][:, h * 128:(h + 1) * 128], st6[:, 1 + h, 1:2], sgt[:, h * 128:(h + 1) * 128], ALU.mult, ALU.mult)
        nct = i // 4 + 1
        ncols = 8 * i + 8
        njt = (ncols + 127) // 128
        for g in range(2):
            gs = slice(64 * g, 64 * g + 64)
            zinit(2, 0, 260)
            zinit(3, 0, 512)
            zinit(4, 0, 512)
            items = []
            for ct in range(nct):
                masks = []
                if ct == nct - 1:
                    masks = [(identb, mcore[:, 12 + (i % 4), :], (t_cb, t_mc), False)]
                items.append(dict(g=g, kT=KCT[:, ct * 128:(ct + 1) * 128], vA=VCA[:, ct, g, :], masks=masks,
                                  rds=(t_kc[ct],), acc=2, last=(ct == nct - 1), imp_ct=ct))
            run_pipeline(items)
            finish(g, 2, 0)
            gate_out(g, 2, 0)
            for r in range(4):
                bank = 3 + r // 2
                base = (r % 2) * 256
                c_ = (g * 4 + r) * 3 + 0
                if r == 0:
                    V("dve", "tensor_scalar_mul", (pst[bank], t_rd), (t_sc,), sc[:, 0:ncols],
                      PS[bank][:, base:base + ncols], rden[:, c_:c_ + 1])
                else:
                    V("dve", "scalar_tensor_tensor", (pst[bank], t_rd, t_sc), (t_sc,), sc[:, 0:ncols],
                      PS[bank][:, base:base + ncols], rden[:, c_:c_ + 1], sc[:, 0:ncols], ALU.mult, ALU.add)
            if i == 0:
                V("dve", "tensor_add", (t_sc, t_vc), (t_sc,), sc[:, 0:8], sc[:, 0:8], vcore[:, 9:17])
            else:
                V("dve", "tensor_add", (t_sc, t_vc), (t_sc,), sc[:, ncols - 9:ncols], sc[:, ncols - 9:ncols],
                  vcore[:, 0:9])
                V("dve", "tensor_scalar_add", (t_sc,), (t_sc,), sc[:, 0:1], sc[:, 0:1], 1.0e4)
            V("dve", "max", (t_sc,), (t_sc,), m8[:, 0:8], sc[:, 0:ncols])
            V("dve", "match_replace", (t_sc,), (t_sc,), sc2[:, 0:ncols], m8[:, 0:8], sc[:, 0:ncols], -1.0e30)
            V("dve", "max", (t_sc,), (t_sc,), m8[:, 0:8], sc2[:, 0:ncols])
            V("dve", "tensor_scalar", (t_sc,), (t_nsel,), nsel[:, 0:ncols], sc[:, 0:ncols], m8[:, 7:8], NEG,
              ALU.is_lt, ALU.mult)
            if debug.get("nsel") is not None and i == debug.get("_i", 0):
                V("dve", "tensor_copy", (t_nsel,), (t_sc,), sc2[:, 0:ncols], nsel[:, 0:ncols])
                P.dma(dbg["nsel"][g * 128:(g + 1) * 128, 0:ncols], sc2[:, 0:ncols], (t_sc,), ())
            for jt in range(njt):
                w_ = 128
                tr(PSb5[0:w_, jt * 128:(jt + 1) * 128], nsel[:, jt * 128:jt * 128 + w_], identb, (t_nsel, t_cb),
                   (pst[5],))
                V("dve", "tensor_copy", (pst[5],), (t_nselT[g],), nselT[0:w_, g, jt, :, :],
                  PSb5[0:w_, jt * 128:(jt + 1) * 128].unsqueeze(1).broadcast_to([w_, 4, 128]))
        zinit(6, 0, 260)
        zinit(7, 0, 260)
        slots = {}

        def fetch(qd):
            rs_ = rcnt[0] % NRING
            rcnt[0] += 1
            slots[qd] = rs_
            P.dma(kring[rs_][:, 0:512], ksT_d[:, qd * 512:(qd + 1) * 512], tuple(t_ks[4 * qd:4 * qd + 4]), (t_ring[rs_],))
            P.dma(kring[rs_][:, 512:1032], vsA_d[:, qd * 520:(qd + 1) * 520], tuple(t_ks[4 * qd:4 * qd + 4]),
                  (t_ring[rs_],))

        for qd in range(min(2, i + 1)):
            fetch(qd)
        items = []
        for qd in range(i + 1):
            for g in range(2):
                gs = slice(64 * g, 64 * g + 64)
                for kk in range(4):
                    kt = 4 * qd + kk
                    j0 = 2 * kt
                    jt, v_ = j0 // 128, (j0 % 128) // 2
                    it = dict(g=g, acc=6 + g, last=(qd == i and kk == 3), qd=qd, kk=kk, gs=gs)
                    it["mk"] = [(ex32[:, v_, :], nselT[:, g, jt, :, :].rearrange("p a b -> p (a b)"), (t_ex, t_nselT[g]), True)]
                    if kt >= 4 * i:
                        it["mk"].append((identb, mcore[:, kt - 4 * i, :], (t_cb, t_mc), False))
                    if g == 0 and kk == 0 and qd + 2 <= i:
                        it["pre"] = (lambda q=qd + 2: fetch(q))
                    items.append(it)
        def prep_sel(it):
            rs_ = slots[it["qd"]]
            kk = it["kk"]; g = it["g"]
            it["kT"] = kring[rs_][:, kk * 128:(kk + 1) * 128]
            it["vA"] = kring[rs_][:, 512 + kk * 130 + g * 65:512 + kk * 130 + (g + 1) * 65]
            it["masks"] = it["mk"]
            it["rds"] = (t_ring[rs_],)
            it["sbanks"] = (0, 1, 3)

        cad = max(1, len(items) // 52)
        bgc = [0]

        def bgstep():
            bgc[0] += 1
            if bg is not None and bgc[0] % cad == 0:
                next(bg, None)
        run_pipeline(items, depth=2, prep=prep_sel, bgstep=bgstep)
        if bg is not None:
            for _ in bg:
                pass
        for g in range(2):
            finish(g, 6 + g, 1)
            gate_out(g, 6 + g, 1)
        for g in range(2):
            gs = slice(64 * g, 64 * g + 64)
            zinit(2, 0, 260)
            jjs = [jj for jj in range(8) if 4 * i - 4 + jj >= 0]
            items = []
            for jj in jjs:
                kt = 4 * i - 4 + jj
                slot = kt % 16
                masks = [(identb, mcore[:, 4 + jj, :], (t_cb, t_mc), False)]
                items.append(dict(g=g, kT=kwR[:, slot, :], vA=vwR[:, slot, g, :], masks=masks, rds=(t_kw[slot],),
                                  acc=2, last=(jj == jjs[-1]), sbanks=(0, 1, 3, 4)))
            run_pipeline(items, depth=2)
            finish(g, 2, 2)
            gate_out(g, 2, 2)
        V("dve", "bn_stats", (t_on,), (t_st,), st6[:, 0, :], onsa[:, :])
        V("dve", "bn_aggr", (t_st,), (t_st,), mv[:, 0, :], st6[:, 0, :])
        V("dve", "tensor_mul", (t_st,), (t_st,), mv[:, 1, 0:1], mv[:, 0, 0:1], mv[:, 0, 0:1])
        V("dve", "tensor_add", (t_st,), (t_st,), mv[:, 1, 0:1], mv[:, 1, 0:1], mv[:, 0, 1:2])
        act(mv[:, 1, 1:2], mv[:, 1, 0:1], AF.Ln, (t_st,), (t_st,), bias=eps6[:, 0:1])
        act(mv[:, 1, 1:2], mv[:, 1, 1:2], AF.Exp, (t_st,), (t_st,), scale=-0.5)
        V("dve", "scalar_tensor_tensor", (t_on, t_st, t_W), (t_mixn,), mixn[:, 0:512], onsa[:, :], mv[:, 1, 1:2],
          nsag[:, :], ALU.mult, ALU.mult)
        if debug.get("onsa") is not None:
            P.dma(dbg["onsa"][i * 128:(i + 1) * 128, :], onsa[:, :], (t_on,), ())

        mp = i % 2
        for ft in range(8):
            tr(PSb5[:, ft * 128:(ft + 1) * 128], mixn[:, ft * 128:(ft + 1) * 128], identb, (t_mixn, t_cb), (pst[5],))
        act(mixT[mp][:, :, :], PSb5[:, :].rearrange("p (a b) -> p a b", a=8), AF.Copy, (pst[5],), (t_mixT[mp],))
        P.dma(mixT_d[i, :, :], mixT[mp][:, :, :].rearrange("p a b -> p (a b)"), (t_mixT[mp],), (t_mixd[i],))

    t_mixd = [Tok() for _ in range(NOWN)]
    t_x2d = [Tok() for _ in range(NOWN)]
    PSb5 = PS[5][:, :].bitcast(BF16)

    kc = [0]

    def sup_gen(i2):
        for jj in range(4):
            k = kc[0]
            kc[0] += 1
            yield from full_tile(4 * i2 + jj, k)

    for i2 in range(min(2, NSUP)):
        for _ in sup_gen(i2):
            pass
    for i in range(NSUP):
        compress(i)
        bg = sup_gen(i + 2) if i + 2 < NSUP else None
        k = kc[0]
        kc[0] += 1
        own_tile(i, k, bg)
        if bg is not None:
            for _ in bg:
                pass
    if "stopA" in debug:
        P.emit()
        return nc

    P.barrier()
    sb = SB(nc)
    cfB = sb.t([128, 128], F32, "cfB"); cbB = sb.t([128, 128], BF16, "cbB"); t_cB = Tok()
    P.dma(cfB[:, :], c_f32[:, 0:128], (), (t_cB,))
    P.dma(cbB[:, :], c_f32[:, 0:128], (), (t_cB,), eng="pool")
    epsB = sb.t([128, 2], F32, "epsB")
    V("pool", "memset", (), (t_cB,), epsB[:, 0:1], 1e-5)
    onesB = sb.t([128, 2], BF16, "onesB")
    V("pool", "memset", (), (t_cB,), onesB[:, :], 1.0)
    lnc = sb.t([128, 6 * D], F32, "lnc"); t_ln = Tok()
    P.dma(lnc[:, :], ln_bc, (), (t_ln,))
    WB_ = {}
    t_WB = Tok()
    for nm, src in (("out", w_out), ("q", wq), ("o", wo), ("k", wk), ("v", wv)):
        WB_[nm] = sb.t([128, 8, D], BF16, "W" + nm)
        sv = src.rearrange("(kt p) c -> p kt c", p=128)
        for kt in range(0, 8, 2):
            P.dma(WB_[nm][:, kt:kt + 2, :], sv[:, kt:kt + 2, :], (), (t_WB,), eng="pool")
    NBLK_ = (NOWN * 256 + 32 * 512) // 128
    xs_d = nc.dram_tensor("xs_d", [NBLK_ * 128, D], BF16, kind="Internal").ap()
    zt = sb.t([128, D], BF16, "zt"); t_zt = Tok(); t_xsz = Tok()
    V("pool", "memset", (), (t_zt,), zt[:, :], 0.0)
    for zz in range(NBLK_ // 4):
        P.dma(xs_d[zz * 512:(zz + 1) * 512, :].rearrange("(p k) d -> p k d", p=128),
              zt[:, :].unsqueeze(1).broadcast_to([128, 4, D]), (t_zt,), (t_xsz,))
    xsB = [sb.t([128, D], F32, "xsB") for _ in range(2)]; t_xsB = [Tok(), Tok()]
    memT = sb.t([128, 8, 256], BF16, "memT"); t_memT = Tok()
    memKT = sb.t([128, 8, 256], BF16, "memKT"); memV = sb.t([128, 2, D], BF16, "memV"); t_mem = Tok()
    for mt in range(2):
        P.dma(xsB[mt][:, :], mem[mt * 128:(mt + 1) * 128, :], (), (t_xsB[mt],))
        for half in range(2):
            for q in range(4):
                kt = half * 4 + q
                tr(PS[half][:, q * 128:(q + 1) * 128], xsB[mt][:, kt * 128:(kt + 1) * 128], cfB[:, :],
                   (t_xsB[mt], t_cB), (pst[half],))
            act(memT[:, half * 4:half * 4 + 4, mt * 128:(mt + 1) * 128],
                PS[half][:, :].rearrange("p (a b) -> p a b", a=4), AF.Copy, (pst[half],), (t_memT,))
    for ft in range(8):
        bank = 2 + ft % 2
        for kt in range(8):
            mm(PS[bank][:, 0:256], WB_["k"][:, kt, ft * 128:(ft + 1) * 128], memT[:, kt, :], (t_WB, t_memT),
               (pst[bank],), start=(kt == 0), stop=(kt == 7))
        act(memKT[:, ft, :], PS[bank][:, 0:256], AF.Copy, (pst[bank],), (t_mem,))
    for mt in range(2):
        for half in range(2):
            bank = 4 + half
            for kt in range(8):
                mm(PS[bank][:, :], memT[:, kt, mt * 128:(mt + 1) * 128], WB_["v"][:, kt, half * 512:(half + 1) * 512],
                   (t_WB, t_memT), (pst[bank],), start=(kt == 0), stop=(kt == 7))
            V("dve", "tensor_copy", (pst[bank],), (t_mem,), memV[:, mt, half * 512:(half + 1) * 512], PS[bank][:, :])

    mixTt = [sb.t([128, 8, 128], BF16, "mixTt") for _ in range(2)]; t_mx = [Tok(), Tok()]
    y1 = sb.t([128, D], F32, "y1"); t_y1 = Tok()
    x1 = sb.t([128, D], F32, "x1"); t_x1 = Tok()
    x1T = sb.t([128, 8, 128], BF16, "x1T"); t_x1T = Tok()
    qxT = sb.t([128, 8, 128], BF16, "qxT"); t_qx = Tok()
    PTb = sb.t([128, 8, 128], BF16, "PTb"); t_PT = Tok()
    on_ = sb.t([128, D], BF16, "on_"); t_onb = Tok()
    oTb = sb.t([128, 8, 128], BF16, "oTb"); t_oT = Tok()
    y2 = sb.t([128, D], F32, "y2"); t_y2 = Tok()
    x2 = [sb.t([128, D], F32, "x2") for _ in range(2)]; t_x2 = [Tok(), Tok()]
    stB = sb.t([128, 2, 6], F32, "stB"); mvB = sb.t([128, 4], F32, "mvB"); t_stB = Tok()
    rdB = sb.t([128, 4], F32, "rdB"); t_rdB = Tok()
    PSb2 = PS[2][:, :].bitcast(BF16)
    print("phase B SBUF end:", sb.cur)

    def layer_norm(src, t_src, dst, t_dst, gi, stt, mvt, t_s, epst):
        for half in range(2):
            V("dve", "bn_stats", (t_src,), (t_s,), stt[:, half, :], src[:, half * 512:(half + 1) * 512])
        V("dve", "bn_aggr", (t_s,), (t_s,), mvt[:, 0:2], stt[:, :, :].rearrange("p a b -> p (a b)"))
        act(mvt[:, 2:3], mvt[:, 1:2], AF.Ln, (t_s,), (t_s,), bias=epst[:, 0:1])
        act(mvt[:, 2:3], mvt[:, 2:3], AF.Exp, (t_s,), (t_s,), scale=-0.5)
        V("dve", "tensor_scalar", (t_src, t_s), (t_dst,), dst[:, :], src[:, :], mvt[:, 0:1], mvt[:, 2:3],
          ALU.subtract, ALU.mult)
        V("dve", "tensor_mul", (t_dst, t_ln), (t_dst,), dst[:, :], dst[:, :], lnc[:, gi * D:(gi + 1) * D])
        V("dve", "tensor_add", (t_dst, t_ln), (t_dst,), dst[:, :], dst[:, :], lnc[:, (gi + 1) * D:(gi + 2) * D])

    for i in range(NOWN):
        p = i % 2
        P.dma(mixTt[p][:, :, :].rearrange("p a b -> p (a b)"), mixT_d[i, :, :], (t_mixd[i],), (t_mx[p],))
        P.dma(xsB[p][:, :], xown[i * 128:(i + 1) * 128, :], (), (t_xsB[p],))
        for half in range(2):
            for kt in range(8):
                mm(PS[half][:, :], mixTt[p][:, kt, :], WB_["out"][:, kt, half * 512:(half + 1) * 512], (t_mx[p], t_WB),
                   (pst[half],), start=(kt == 0), stop=(kt == 7))
            V("dve", "scalar_tensor_tensor", (t_xsB[p], pst[half]), (t_y1,), y1[:, half * 512:(half + 1) * 512],
              xsB[p][:, half * 512:(half + 1) * 512], ALPHA, PS[half][:, :], ALU.mult, ALU.add)
        layer_norm(y1, t_y1, x1, t_x1, 0, stB, mvB, t_stB, epsB)
        if debug.get("x1") is not None:
            P.dma(dbg["x1"][i * 128:(i + 1) * 128, :], x1[:, :], (t_x1,), ())
        for half in range(2):
            for q in range(4):
                kt = half * 4 + q
                tr(PS[half][:, q * 128:(q + 1) * 128], x1[:, kt * 128:(kt + 1) * 128], cfB[:, :], (t_x1, t_cB),
                   (pst[half],))
            act(x1T[:, half * 4:half * 4 + 4, :], PS[half][:, :].rearrange("p (a b) -> p a b", a=4), AF.Copy,
                (pst[half],), (t_x1T,))
        for ft in range(8):
            bank = 2 + ft // 4
            for kt in range(8):
                mm(PS[bank][:, (ft % 4) * 128:(ft % 4 + 1) * 128], WB_["q"][:, kt, ft * 128:(ft + 1) * 128], x1T[:, kt, :],
                   (t_WB, t_x1T), (pst[bank],), start=(kt == 0), stop=(kt == 7))
        for hb in range(2):
            act(qxT[:, hb * 4:hb * 4 + 4, :], PS[2 + hb][:, :].rearrange("p (a b) -> p a b", a=4), AF.Copy,
                (pst[2 + hb],), (t_qx,), scale=1.0 / 16.0)
        for hd in range(4):
            for mt in range(2):
                r8 = hd * 2 + mt
                bank = 4 + r8 // 4
                for k2 in range(2):
                    ft = hd * 2 + k2
                    mm(PS[bank][:, (r8 % 4) * 128:(r8 % 4 + 1) * 128], memKT[:, ft, mt * 128:(mt + 1) * 128], qxT[:, ft, :],
                       (t_mem, t_qx), (pst[bank],), start=(k2 == 0), stop=(k2 == 1))
        for hb in range(2):
            act(PTb[:, hb * 4:hb * 4 + 4, :], PS[4 + hb][:, :].rearrange("p (a b) -> p a b", a=4), AF.Exp,
                (pst[4 + hb],), (t_PT,))
        for hd in range(4):
            bank = 6 + hd // 2
            for mt in range(2):
                mm(PS[bank][:, (hd % 2) * 256:(hd % 2 + 1) * 256], PTb[:, hd * 2 + mt, :], memV[:, mt, hd * 256:(hd + 1) * 256],
                   (t_PT, t_mem), (pst[bank],), start=(mt == 0), stop=(mt == 1))
        for hd in range(4):
            for mt in range(2):
                mm(PS[0][:, hd:hd + 1], PTb[:, hd * 2 + mt, :], onesB[:, 0:1], (t_PT, t_cB), (pst[0],), start=(mt == 0),
                   stop=(mt == 1))
        V("dve", "reciprocal", (pst[0],), (t_rdB,), rdB[:, :], PS[0][:, 0:4])
        for hd in range(4):
            bank = 6 + hd // 2
            V("dve", "tensor_scalar_mul", (pst[bank], t_rdB), (t_onb,), on_[:, hd * 256:(hd + 1) * 256],
              PS[bank][:, (hd % 2) * 256:(hd % 2 + 1) * 256], rdB[:, hd:hd + 1])
        for ft in range(8):
            tr(PSb2[:, ft * 128:(ft + 1) * 128], on_[:, ft * 128:(ft + 1) * 128], cbB[:, :], (t_onb, t_cB), (pst[2],))
        act(oTb[:, :, :], PSb2[:, :].rearrange("p (a b) -> p a b", a=8), AF.Copy, (pst[2],), (t_oT,))
        for half in range(2):
            for kt in range(8):
                mm(PS[half][:, :], oTb[:, kt, :], WB_["o"][:, kt, half * 512:(half + 1) * 512], (t_oT, t_WB),
                   (pst[half],), start=(kt == 0), stop=(kt == 7))
            V("dve", "scalar_tensor_tensor", (t_x1, pst[half]), (t_y2,), y2[:, half * 512:(half + 1) * 512],
              x1[:, half * 512:(half + 1) * 512], ALPHA, PS[half][:, :], ALU.mult, ALU.add)
        layer_norm(y2, t_y2, x2[p], t_x2[p], 2, stB, mvB, t_stB, epsB)
        P.dma(x2_d[i * 128:(i + 1) * 128, :], x2[p][:, :], (t_x2[p],), (t_x2d[i],))
        if debug.get("x2") is not None:
            P.dma(dbg["x2"][i * 128:(i + 1) * 128, :], x2[p][:, :], (t_x2[p],), ())
    if "stopB" in debug:
        P.emit()
        return nc

    P.barrier()
    sb = SB(nc)
    AXX = mybir.AxisListType.X
    cfC = sb.t([128, 128], F32, "cfC"); t_cC = Tok()
    P.dma(cfC[:, :], c_f32[:, 0:128], (), (t_cC,))
    epsC = sb.t([128, 2], F32, "epsC")
    V("pool", "memset", (), (t_cC,), epsC[:, 0:1], 1e-5)
    ln3 = sb.t([128, 2 * D], F32, "ln3"); t_ln = Tok()
    P.dma(ln3[:, :], ln_bc[:, 4 * D:6 * D], (), (t_ln,))
    Wr = sb.t([128, 8, 36], F32, "Wr"); brt = sb.t([128, 36], F32, "brt"); t_wr = Tok()
    P.dma(Wr[:, :, :], mo_wr.rearrange("(kt p) c -> p kt c", p=128), (), (t_wr,))
    P.dma(brt[:, :], mo_br, (), (t_wr,))
    I32 = mybir.dt.int32
    BS = 128
    NBLK = (NOWN * 256 + 32 * BS) // BS
    NSLOT = NBLK * BS
    ys_d = nc.dram_tensor("ys_d", [NSLOT, D], F32, kind="Internal").ap()
    cbC = sb.t([128, 3, 128], BF16, "cbC")
    P.dma(cbC[:, 0, :], c_f32[:, 0:128], (), (t_cC,), eng="pool")
    P.dma(cbC[:, 1, :], c_moe[:, 0:128], (), (t_cC,), eng="pool")
    V("pool", "memset", (), (t_cC,), cbC[:, 2, :], 1.0)
    cmo = sb.t([128, 12 + 192], F32, "cmo"); t_cmo = Tok()
    P.dma(cmo[:, :], c_moe[:, 128:128 + 12 + 192], (), (t_cmo,))
    base_rel = sb.t([128, 32], F32, "base_rel"); t_base = Tok()
    V("pool", "memset", (), (t_base,), base_rel[:, :], 0.0)
    oh_all = sb.t([128, NOWN, 2, 32], F32, "oh_all"); rank_all = sb.t([128, NOWN, 2], F32, "rank_all")
    dest_f = sb.t([128, NOWN, 2], F32, "dest_f")
    dest_i = sb.t([128, NOWN, 2], I32, "dest_i"); wts = sb.t([128, NOWN, 2], F32, "wts")
    t_dst = [Tok() for _ in range(NOWN)]
    xsC = [sb.t([128, D], F32, "xsC") for _ in range(2)]; t_xsC = [Tok(), Tok()]
    x2b = [sb.t([128, D], BF16, "x2b") for _ in range(2)]; t_x2b = [Tok(), Tok()]
    x2Tf = sb.t([128, 8, 128], F32, "x2Tf"); t_x2f = Tok()
    lg = sb.t([128, 36], F32, "lg"); elm = sb.t([128, 32], F32, "elm"); ohx = sb.t([128, 2, 32], F32, "ohx")
    ohs = sb.t([128, 32], BF16, "ohs"); pos = sb.t([128, 32], F32, "pos")
    rs = sb.t([128, 20], F32, "rs"); t_r = Tok()
    for tg in range(NOWN):
        b_ = tg % 2
        P.dma(xsC[b_][:, :], x2_d[tg * 128:(tg + 1) * 128, :], (t_x2d[tg],), (t_xsC[b_],))
        for half in range(2):
            for q in range(4):
                kt = half * 4 + q
                tr(PS[half][:, q * 128:(q + 1) * 128], xsC[b_][:, kt * 128:(kt + 1) * 128], cfC[:, :],
                   (t_xsC[b_], t_cC), (pst[half],))
            V("dve", "tensor_copy", (pst[half],), (t_x2f,), x2Tf[:, half * 4:half * 4 + 4, :],
              PS[half][:, :].rearrange("p (a b) -> p a b", a=4))
        for kt in range(8):
            mm(PS[2][:, 0:36], x2Tf[:, kt, :], Wr[:, kt, :], (t_x2f, t_wr), (pst[2],), start=(kt == 0), stop=(kt == 7))
        V("dve", "tensor_add", (pst[2], t_wr), (t_r,), lg[:, :], PS[2][:, 0:36], brt[:, :])
        V("dve", "reduce_max", (t_r,), (t_r,), rs[:, 0:1], lg[:, 0:4], AXX)
        V("dve", "tensor_scalar_mul", (t_r,), (t_r,), rs[:, 1:2], rs[:, 0:1], -1.0)
        act(ohx[:, 0, 0:4], lg[:, 0:4], AF.Exp, (t_r,), (t_r,), bias=rs[:, 1:2], accum_out=rs[:, 2:3])
        V("dve", "reciprocal", (t_r,), (t_r,), rs[:, 3:4], rs[:, 2:3])
        V("dve", "tensor_scalar", (t_r,), (t_r,), rs[:, 4:8], lg[:, 0:4], rs[:, 0:1], 1.0e30, ALU.is_equal, ALU.mult)
        V("dve", "tensor_scalar_add", (t_r,), (t_r,), rs[:, 4:8], rs[:, 4:8], -1.0e30)
        V("dve", "tensor_add", (t_r,), (t_r,), elm[:, :].rearrange("p (g e) -> p g e", g=4),
          lg[:, 4:36].rearrange("p (g e) -> p g e", g=4), rs[:, 4:8].unsqueeze(2).broadcast_to([128, 4, 8]))
        V("dve", "max", (t_r,), (t_r,), rs[:, 8:16], elm[:, :])
        V("dve", "tensor_sub", (t_r,), (t_r,), rs[:, 2:3], rs[:, 9:10], rs[:, 8:9])
        act(rs[:, 2:3], rs[:, 2:3], AF.Exp, (t_r,), (t_r,))
        V("dve", "tensor_scalar_add", (t_r,), (t_r,), rs[:, 2:3], rs[:, 2:3], 1.0)
        V("dve", "reciprocal", (t_r,), (t_r,), rs[:, 2:3], rs[:, 2:3])
        V("dve", "tensor_mul", (t_r,), (t_dst[tg],), wts[:, tg, 0:1], rs[:, 2:3], rs[:, 3:4])
        V("dve", "tensor_sub", (t_r, t_dst[tg]), (t_dst[tg],), wts[:, tg, 1:2], rs[:, 3:4], wts[:, tg, 0:1])
        V("dve", "tensor_scalar", (t_r,), (t_dst[tg],), oh_all[:, tg, 0, :], elm[:, :], rs[:, 8:9], None, ALU.is_equal)
        V("dve", "tensor_scalar", (t_r,), (t_dst[tg],), oh_all[:, tg, 1, :], elm[:, :], rs[:, 9:10], None, ALU.is_equal)
        V("dve", "tensor_add", (t_dst[tg],), (t_r,), ohs[:, :], oh_all[:, tg, 0, :], oh_all[:, tg, 1, :])
        mm(PS[3][:, 0:32], cbC[:, 1, :], ohs[:, :], (t_cC, t_r), (pst[3],))
        mm(PS[3][:, 32:64], cbC[:, 2, :], ohs[:, :], (t_cC, t_r), (pst[3],))
        V("dve", "tensor_add", (pst[3], t_base), (t_r,), pos[:, :], PS[3][:, 0:32], base_rel[:, :])
        V("dve", "tensor_add", (pst[3], t_base), (t_base,), base_rel[:, :], base_rel[:, :], PS[3][:, 32:64])
        for k2 in range(2):
            V("dve", "tensor_mul", (t_r, t_dst[tg]), (t_r,), ohx[:, k2, :], oh_all[:, tg, k2, :], pos[:, :])
            V("dve", "reduce_sum", (t_r,), (t_dst[tg],), rank_all[:, tg, k2:k2 + 1], ohx[:, k2, :], AXX)
    pad = sb.t([128, 4, 32], F32, "pad"); t_pad = Tok()
    NTH = 8192 // BS
    cm2 = sb.t([128, 32, NTH], F32, "cm2")
    V("dve", "tensor_tensor", (t_base, t_cmo), (t_pad,), cm2[:, :, :], base_rel[:, :].unsqueeze(2).broadcast_to([128, 32, NTH]),
      cmo[:, 12:12 + (BS // 128) * NTH:BS // 128].unsqueeze(1).broadcast_to([128, 32, NTH]), ALU.is_gt)
    V("dve", "reduce_sum", (t_pad,), (t_pad,), pad[:, 0, :], cm2[:, :, :], AXX)
    V("dve", "tensor_scalar_mul", (t_pad,), (t_pad,), pad[:, 1, :], pad[:, 0, :], float(BS))
    V("pool", "memset", (), (t_pad,), pad[:, 3, :], 1.0)
    V("dve", "tensor_tensor_scan", (t_pad,), (t_pad,), pad[:, 2, :], pad[:, 3, :], pad[:, 1, :], 0.0, ALU.mult, ALU.add)
    V("dve", "tensor_sub", (t_pad,), (t_pad,), pad[:, 0, :], pad[:, 2, :], pad[:, 1, :])
    for tg in range(NOWN):
        for k2 in range(2):
            V("dve", "tensor_mul", (t_pad, t_dst[tg]), (t_r,), ohx[:, k2, :], oh_all[:, tg, k2, :], pad[:, 0, :])
            V("dve", "reduce_sum", (t_r,), (t_r,), rs[:, 16 + k2:17 + k2], ohx[:, k2, :], AXX)
        V("dve", "tensor_add", (t_r, t_dst[tg]), (t_dst[tg],), dest_f[:, tg, :], rs[:, 16:18], rank_all[:, tg, :])
        V("dve", "tensor_copy", (t_dst[tg],), (t_dst[tg],), dest_i[:, tg, :], dest_f[:, tg, :])
        if debug.get("wt") is not None:
            P.dma(dbg["wt"][tg * 128:(tg + 1) * 128, 0:2], dest_f[:, tg, :], (t_dst[tg],), ())
            P.dma(dbg["wt"][tg * 128:(tg + 1) * 128, 2:4], wts[:, tg, :], (t_dst[tg],), ())
    cmpb = sb.t([128, NBLK, 32], F32, "cmpb"); bex = sb.t([128, NBLK], F32, "bex"); t_be = Tok()
    V("dve", "tensor_tensor", (t_pad, t_cmo), (t_be,), cmpb[:, :, :], pad[:, 2, :].unsqueeze(1).broadcast_to([128, NBLK, 32]),
      cmo[:, 12:12 + (BS // 128) * NBLK:BS // 128].unsqueeze(2).broadcast_to([128, NBLK, 32]), ALU.is_le)
    V("dve", "reduce_sum", (t_be,), (t_be,), bex[:, :], cmpb[:, :, :], AXX)
    V("dve", "tensor_scalar_min", (t_be,), (t_be,), bex[:, :], bex[:, :], 31.0)
    idxf = sb.t([128, NBLK, 6], F32, "idxf"); idxi = sb.t([128, NBLK, 6], I32, "idxi"); t_idx = Tok()
    V("dve", "scalar_tensor_tensor", (t_be, t_cmo), (t_idx,), idxf[:, :, 0:4], bex[:, :].unsqueeze(2).broadcast_to([128, NBLK, 4]),
      512.0, cmo[:, 0:4].unsqueeze(1).broadcast_to([128, NBLK, 4]), ALU.mult, ALU.add)
    V("dve", "scalar_tensor_tensor", (t_be, t_cmo), (t_idx,), idxf[:, :, 4:6], bex[:, :].unsqueeze(2).broadcast_to([128, NBLK, 2]),
      256.0, cmo[:, 0:2].unsqueeze(1).broadcast_to([128, NBLK, 2]), ALU.mult, ALU.add)
    V("dve", "tensor_copy", (t_idx,), (t_idx,), idxi[:, :, :], idxf[:, :, :])
    t_scat = [Tok() for _ in range(NOWN)]
    for tg in range(NOWN):
        b_ = tg % 2
        P.dma(xsC[b_][:, :], x2_d[tg * 128:(tg + 1) * 128, :], (t_x2d[tg],), (t_xsC[b_],))
        act(x2b[b_][:, :], xsC[b_][:, :], AF.Copy, (t_xsC[b_],), (t_x2b[b_],))
        for k2 in range(2):
            P.op("pool", (lambda e, tg=tg, k2=k2, b_=b_: e.indirect_dma_start(
                out=xs_d[:, :], out_offset=bass.IndirectOffsetOnAxis(ap=dest_i[:, tg, k2:k2 + 1], axis=0),
                in_=x2b[b_][:, :], in_offset=None)),
                (t_x2b[b_], t_dst[tg]), (t_scat[tg],), dma=True)
    WGUb = [sb.t([128, 8, 1024], BF16, "WGUb") for _ in range(2)]
    WDb = [sb.t([128, 4, D], BF16, "WDb") for _ in range(2)]
    t_wgu = [[Tok() for _ in range(4)] for _ in range(2)]
    t_wd = [[Tok() for _ in range(2)] for _ in range(2)]
    xe = [sb.t([128, D], BF16, "xe") for _ in range(2)]; t_xe = [Tok(), Tok()]
    xeT = [sb.t([128, 8, 128], BF16, "xeT") for _ in range(2)]; t_xeT = [Tok(), Tok()]
    sgC = sb.t([128, 512], F32, "sgC"); t_sg = Tok()
    hb = sb.t([128, 512], BF16, "hb"); t_hb = Tok()
    hTb = [sb.t([128, 4, 128], BF16, "hTb") for _ in range(2)]; t_hT = [Tok(), Tok()]
    ysb = [sb.t([128, D], F32, "ysb") for _ in range(2)]; t_ysb = [Tok(), Tok()]
    t_ysd = [Tok() for _ in range(NBLK)]
    PSbC = [PS[6][:, :].bitcast(BF16), PS[7][:, :].bitcast(BF16)]

    def wload(blk):
        wb_ = blk % 2
        for kp in range(4):
            P.op("pool", (lambda e, blk=blk, kp=kp, wb_=wb_: e.indirect_dma_start(
                out=WGUb[wb_][:, 2 * kp:2 * kp + 2, :].rearrange("p a b -> p (a b)"), out_offset=None, in_=mo_wgu[:, :],
                in_offset=bass.IndirectOffsetOnAxis(ap=idxi[:, blk, kp:kp + 1], axis=0))),
                (t_idx,), (t_wgu[wb_][kp],), dma=True)
        for fp in range(2):
            P.op("pool", (lambda e, blk=blk, fp=fp, wb_=wb_: e.indirect_dma_start(
                out=WDb[wb_][:, 2 * fp:2 * fp + 2, :].rearrange("p a b -> p (a b)"), out_offset=None, in_=mo_wd2[:, :],
                in_offset=bass.IndirectOffsetOnAxis(ap=idxi[:, blk, 4 + fp:5 + fp], axis=0))),
                (t_idx,), (t_wd[wb_][fp],), dma=True)

    sg2 = [sgC, sb.t([128, 512], F32, "sgC2")]; t_sg2 = [t_sg, Tok()]
    hb2 = [hb, sb.t([128, 512], BF16, "hb2")]; t_hb2 = [t_hb, Tok()]
    NSTB = BS // 128
    NTIL = NBLK * NSTB
    PSb3 = PS[3][:, :].bitcast(BF16)

    def stA(sidx):
        xb_ = sidx % 2
        P.dma(xe[xb_][:, :], xs_d[sidx * 128:(sidx + 1) * 128, :], tuple(t_scat), (t_xe[xb_],))
        pb_ = 6 + xb_
        for kt in range(8):
            tr(PSbC[xb_][:, kt * 128:(kt + 1) * 128], xe[xb_][:, kt * 128:(kt + 1) * 128], cbC[:, 0, :],
               (t_xe[xb_], t_cC), (pst[pb_],))
        act(xeT[xb_][:, :, :], PSbC[xb_][:, :].rearrange("p (a b) -> p a b", a=8), AF.Copy, (pst[pb_],), (t_xeT[xb_],))

    def stB(sidx):
        blk = sidx // NSTB
        wb_ = blk % 2
        xb_ = sidx % 2
        for kt in range(8):
            mm(PS[0][:, :], xeT[xb_][:, kt, :], WGUb[wb_][:, kt, 0:512], (t_xeT[xb_], t_wgu[wb_][kt // 2]), (pst[0],),
               start=(kt == 0), stop=(kt == 7))
        for kt in range(8):
            mm(PS[1][:, :], xeT[xb_][:, kt, :], WGUb[wb_][:, kt, 512:1024], (t_xeT[xb_], t_wgu[wb_][kt // 2]), (pst[1],),
               start=(kt == 0), stop=(kt == 7))
        act(sg2[xb_][:, :], PS[0][:, :], AF.Silu, (pst[0],), (t_sg2[xb_],))
        V("dve", "tensor_mul", (t_sg2[xb_], pst[1]), (t_hb2[xb_],), hb2[xb_][:, :], sg2[xb_][:, :], PS[1][:, :])
        for ft in range(4):
            tr(PSb3[:, ft * 128:(ft + 1) * 128], hb2[xb_][:, ft * 128:(ft + 1) * 128], cbC[:, 0, :], (t_hb2[xb_], t_cC),
               (pst[3],))
        V("dve", "tensor_copy", (pst[3],), (t_hT[xb_],), hTb[xb_][:, :, :], PSb3[:, 0:512].rearrange("p (a b) -> p a b", a=4))

    def stC(sidx):
        blk = sidx // NSTB
        wb_ = blk % 2
        xb_ = sidx % 2
        for h2 in range(2):
            bank = 4 + h2
            for ft in range(4):
                mm(PS[bank][:, :], hTb[xb_][:, ft, :], WDb[wb_][:, ft, h2 * 512:(h2 + 1) * 512], (t_hT[xb_], t_wd[wb_][ft // 2]),
                   (pst[bank],), start=(ft == 0), stop=(ft == 3))
            if h2 == 0:
                act(ysb[xb_][:, 0:512], PS[bank][:, :], AF.Copy, (pst[bank],), (t_ysb[xb_],))
            else:
                V("dve", "tensor_copy", (pst[bank],), (t_ysb[xb_],), ysb[xb_][:, 512:1024], PS[bank][:, :])
        P.dma(ys_d[sidx * 128:(sidx + 1) * 128, :], ysb[xb_][:, :], (t_ysb[xb_],), (t_ysd[blk],))

    wload(0)
    for t_ in range(NTIL + 2):
        if t_ < NTIL and t_ % NSTB == 0 and t_ // NSTB + 1 < NBLK:
            pass
        if t_ - 2 >= 0:
            stC(t_ - 2)
        if 0 <= t_ - 1 < NTIL:
            if (t_ - 1) % NSTB == 0 and (t_ - 1) // NSTB + 1 < NBLK:
                wload((t_ - 1) // NSTB + 1)
            stB(t_ - 1)
        if t_ < NTIL:
            stA(t_)
    g12 = [sb.t([128, 2, D], F32, "g12") for _ in range(2)]; t_g12 = [Tok(), Tok()]
    y3 = sb.t([128, D], F32, "y3"); t_y3 = Tok()
    o3 = [sb.t([128, D], F32, "o3") for _ in range(2)]; t_o3 = [Tok(), Tok()]
    stC = sb.t([128, 2, 6], F32, "stC"); mvC = sb.t([128, 4], F32, "mvC"); t_stC = Tok()
    print("phase C SBUF end:", sb.cur)
    for tg in range(NOWN):
        b_ = tg % 2
        P.dma(xsC[b_][:, :], x2_d[tg * 128:(tg + 1) * 128, :], (t_x2d[tg],), (t_xsC[b_],))
        for k2 in range(2):
            P.op("pool", (lambda e, tg=tg, k2=k2, b_=b_: e.indirect_dma_start(
                out=g12[b_][:, k2, :], out_offset=None, in_=ys_d[:, :],
                in_offset=bass.IndirectOffsetOnAxis(ap=dest_i[:, tg, k2:k2 + 1], axis=0))),
                tuple(t_ysd) + (t_dst[tg],), (t_g12[b_],), dma=True)
        V("dve", "scalar_tensor_tensor", (t_g12[b_], t_dst[tg]), (t_y3,), y3[:, :], g12[b_][:, 0, :], wts[:, tg, 0:1],
          g12[b_][:, 0, :], ALU.mult, ALU.bypass)
        V("dve", "scalar_tensor_tensor", (t_g12[b_], t_dst[tg], t_y3), (t_y3,), y3[:, :], g12[b_][:, 1, :], wts[:, tg, 1:2],
          y3[:, :], ALU.mult, ALU.add)
        V("dve", "scalar_tensor_tensor", (t_xsC[b_], t_y3), (t_y3,), y3[:, :], xsC[b_][:, :], ALPHA, y3[:, :], ALU.mult,
          ALU.add)
        ob = tg % 2
        for half in range(2):
            V("dve", "bn_stats", (t_y3,), (t_stC,), stC[:, half, :], y3[:, half * 512:(half + 1) * 512])
        V("dve", "bn_aggr", (t_stC,), (t_stC,), mvC[:, 0:2], stC[:, :, :].rearrange("p a b -> p (a b)"))
        act(mvC[:, 2:3], mvC[:, 1:2], AF.Ln, (t_stC,), (t_stC,), bias=epsC[:, 0:1])
        act(mvC[:, 2:3], mvC[:, 2:3], AF.Exp, (t_stC,), (t_stC,), scale=-0.5)
        V("dve", "tensor_scalar", (t_y3, t_stC), (t_o3[ob],), o3[ob][:, :], y3[:, :], mvC[:, 0:1], mvC[:, 2:3],
          ALU.subtract, ALU.mult)
        V("dve", "tensor_mul", (t_o3[ob], t_ln), (t_o3[ob],), o3[ob][:, :], o3[ob][:, :], ln3[:, 0:D])
        V("dve", "tensor_add", (t_o3[ob], t_ln), (t_o3[ob],), o3[ob][:, :], o3[ob][:, :], ln3[:, D:2 * D])
        P.dma(out[tg * 128:(tg + 1) * 128, :], o3[ob][:, :], (t_o3[ob],), ())
    P.emit()
    return nc


_NC_CACHE = {}


def kernel(**inputs):
    x = np.asarray(inputs["x"])
    B, S, _ = x.shape
    if S not in _NC_CACHE:
        _NC_CACHE[S] = build(S)
    nc = _NC_CACHE[S]
    maps = prep(inputs, S)
    res = run_bass_kernel_spmd(nc, maps, core_ids=list(range(8)))
    outp = np.zeros((B, S, D), np.float32)
    ov = outp.reshape(B, S // 512, 4, 128, D)
    for core in range(8):
        b, c = core // 4, core % 4
        ov[b, :, c] = np.asarray(res.results[core]["out"]).reshape(S // 512, 128, D)
    return outp


def _consts():
    s_ = np.arange(128)[:, None]
    t_ = np.arange(128)[None, :]
    same = (s_ // 64) == (t_ // 64)
    ident = np.eye(128, dtype=np.float32)
    msuf = ((s_ > t_) & same).astype(np.float32)
    tri = ((s_ <= t_) & same).astype(np.float32)
    chind = (s_ // 64 == np.arange(2)[None, :]).astype(np.float32)
    c_f32 = np.concatenate([ident, msuf, tri, chind], axis=1)
    nl = np.arange(128)[:, None]
    jl = np.arange(33)[None, :]
    msel = ((4 * jl - 1 <= nl) & (nl <= 4 * jl + 3)).astype(np.float32)
    p = np.arange(128)[:, None, None]
    v = np.arange(64)[None, :, None]
    k = np.arange(128)[None, None, :]
    ex32 = (p == 2 * v + k // 64).astype(np.float32).reshape(128, 64 * 128)
    seg = np.ones((128, 512), np.float32)
    seg[:, ::64] = 0.0
    return c_f32, msel, ex32, seg


def _core_consts(c):
    k = np.arange(128)[:, None]
    tl = np.arange(128)[None, :]
    Z = np.zeros((128, 128), np.float32)
    N = np.full((128, 128), NEG, np.float32)
    caus = np.where(k <= tl, 0.0, NEG).astype(np.float32)
    wst = np.where(k > tl, 0.0, NEG).astype(np.float32)
    cm = [Z if j < c else (caus if j == c else N) for j in range(4)]
    wm = []
    for jj in range(8):
        if jj < c or jj > 4 + c:
            wm.append(N)
        elif jj == c:
            wm.append(wst)
        elif jj == 4 + c:
            wm.append(caus)
        else:
            wm.append(Z)
    cpm = [np.where(16 * k + 31 <= 512 * q + 128 * c + tl, 0.0, NEG).astype(np.float32) for q in range(4)]
    m_core = np.concatenate(cm + wm + cpm, axis=1)
    t = np.arange(128)[:, None]
    cur = 2 * c + (t >= 64)
    j9 = np.arange(-1, 8)[None, :]
    b9 = np.where((j9 == cur) | (j9 == cur - 1), 1.0e4, np.where(j9 > cur, -1.0, 0.0)).astype(np.float32)
    j8 = np.arange(8)[None, :]
    b8 = np.where((j8 == cur) | (j8 == cur - 1) | (j8 == 0), 1.0e4, np.where(j8 > cur, -1.0, 0.0)).astype(np.float32)
    esel = np.zeros((128, 4), np.float32)
    esel[:, c] = 1.0
    return m_core, np.concatenate([b9, b8, esel], axis=1)


def prep(inputs, S):
    f = lambda a: np.ascontiguousarray(np.asarray(a, dtype=np.float32))
    x = f(inputs["x"])
    B = x.shape[0]
    c_f32, msel, ex32, seg = _consts()
    bc = lambda v_: np.ascontiguousarray(np.broadcast_to(v_[None, :], (128, v_.shape[0])))
    lbl = f(inputs["hg_lb_logits"])
    pk = f(inputs["cmp_pe_k"])[0].reshape(16, 2, 64).transpose(1, 2, 0).reshape(128, 16)
    pv = f(inputs["cmp_pe_v"])[0].reshape(16, 2, 64).transpose(1, 2, 0).reshape(128, 16)
    common = {
        "w_in": f(inputs["w_in"])[0],
        "w1k": f(inputs["cmp_w1_k"])[0], "w1v": f(inputs["cmp_w1_v"])[0],
        "w2k": f(inputs["cmp_w2_k"])[0], "w2v": f(inputs["cmp_w2_v"])[0],
        "peT": np.ascontiguousarray(np.concatenate([pk, pv], axis=1)),
        "lbl_bc": np.concatenate([bc(lbl[0]), bc(lbl[1])], axis=1),
        "lbl_km": np.ascontiguousarray(np.concatenate([lbl[0].reshape(4, 128).T, lbl[1].reshape(4, 128).T], axis=1)),
        "nsag_bc": bc(f(inputs["nsa_norm_g"])[0]),
        "hgg_bc": bc(np.tile(f(inputs["hg_norm_g"])[0], 4)),
        "c_f32": c_f32, "c_msel": msel, "c_ex32": ex32, "c_seg": seg,
        "c_moe": np.ascontiguousarray(np.concatenate([
            (np.arange(128)[:, None] < np.arange(128)[None, :]).astype(np.float32),
            (np.arange(8)[None, :] * 128.0 + np.arange(128)[:, None]).astype(np.float32),
            (np.arange(4)[None, :] * 128.0 + np.arange(128)[:, None]).astype(np.float32),
            np.broadcast_to((np.arange(192) * 128.0).astype(np.float32)[None, :], (128, 192))], axis=1)),
        "w_out": f(inputs["w_out"])[0], "xa_wq": f(inputs["xa_wq"])[0], "xa_wk": f(inputs["xa_wk"])[0],
        "xa_wv": f(inputs["xa_wv"])[0], "xa_wo": f(inputs["xa_wo"])[0],
        "ln_bc": np.concatenate([bc(f(inputs[k_])[0]) for k_ in ("ln1_g", "ln1_b", "ln2_g", "ln2_b", "ln3_g", "ln3_b")],
                                axis=1),
        "mo_wr": np.ascontiguousarray(np.concatenate([f(inputs["moe_w_group"])[0], f(inputs["moe_w_expert"])[0]], axis=1)),
        "mo_br": bc(np.concatenate([f(inputs["moe_b_group"])[0], f(inputs["moe_b_expert"])[0]])),
        "mo_wgu": np.ascontiguousarray(np.concatenate([f(inputs["moe_w_gate"])[0], f(inputs["moe_w_up"])[0]], axis=2)
                                       .reshape(32, 4, 2, 128, 1024).transpose(0, 1, 3, 2, 4).reshape(32 * 4 * 128, 2048)),
        "mo_wd2": np.ascontiguousarray(f(inputs["moe_w_down"])[0].reshape(32, 2, 2, 128, 1024).transpose(0, 1, 3, 2, 4)
                                       .reshape(32 * 2 * 128, 2048)),
    }
    maps = []
    for core in range(2 * 4):
        b, c = core // 4, core % 4
        if b >= B:
            b = B - 1
        m_core, v_core = _core_consts(c)
        xb = x[b]
        xo = np.ascontiguousarray(xb.reshape(S // 512, 4, 128, D)[:, c].reshape(-1, D))
        d = dict(common)
        d.update({"xfull": xb, "xown": xo, "mem": f(inputs["mem"])[b], "m_core": m_core, "v_core": v_core})
        maps.append(d)
    return maps
```

```python
import numpy as np
import ml_dtypes
import concourse.bass as bass
import concourse.mybir as mybir
from concourse.bass_utils import run_bass_kernel_spmd

F32 = mybir.dt.float32
BF16 = mybir.dt.bfloat16
AF = mybir.ActivationFunctionType
ALU = mybir.AluOpType

D = 1024
NEG = -30000.0
ALPHA = 2.0 ** 0.25
Q0, KC0, VC0, KS0, VS0, KW0, VW0, G0, HQ0, HF0, HI0, HG0, WEND = (
    0, 512, 640, 768, 896, 1024, 1152, 1280, 1304, 1816, 2328, 2840, 3352)
WB = 768
oKS, oVS, oKW, oVW, oG, oHQ, oHF, oHI, oHG = (KS0 - WB, VS0 - WB, KW0 - WB, VW0 - WB, G0 - WB,
                                              HQ0 - WB, HF0 - WB, HI0 - WB, HG0 - WB)
WN = WEND - WB


class Tok:
    __slots__ = ("name", "w", "rs", "excl")

    def __init__(self, name="", excl=False):
        self.name = name
        self.w = None
        self.rs = []
        self.excl = excl


class Op:
    __slots__ = ("eng", "fn", "deps", "dma", "rank", "sem", "val", "sig", "prev_dma")

    def __init__(self, eng, fn, dma):
        self.eng = eng
        self.fn = fn
        self.dma = dma
        self.deps = []
        self.rank = 0
        self.sem = None
        self.val = 0
        self.sig = False
        self.prev_dma = None


class Prog:
    ENGS = ("pe", "act", "dve", "pool", "sp")

    def __init__(self, nc, n_dma_sems=32, n_pool_sems=16):
        self.nc = nc
        self.ops = {e: [] for e in self.ENGS}
        self.n_dma_sems = n_dma_sems
        self.dma_last = [None] * n_dma_sems
        self.dma_cnt = [0] * n_dma_sems
        self.dma_rr = 0
        self.dma_rr2 = 0
        self.n_pool_sems = n_pool_sems

    def op(self, eng, fn, reads=(), writes=(), dma=False):
        o = Op(eng, fn, dma)
        ex = [t for t in reads if t.excl and t not in writes]
        if ex:
            writes = tuple(writes) + tuple(ex)
        deps = []
        for t in reads:
            if t.w is not None:
                deps.append((t.w, True))
        for t in writes:
            if t.w is not None:
                deps.append((t.w, False))
            for r in t.rs:
                deps.append((r, False))
        fl = []
        seen = {}
        for d, raw in deps:
            if d is o:
                continue
            if id(d) in seen:
                if raw:
                    seen[id(d)][1] = True
                continue
            ent = [d, raw]
            seen[id(d)] = ent
            fl.append(ent)
        out = []
        for d, raw in fl:
            if (not dma) and (not d.dma) and d.eng == eng:
                if eng == "pe":
                    continue
            out.append(d)
        if dma:
            if eng == "pool":
                k = self.n_dma_sems - self.n_pool_sems + self.dma_rr2
                self.dma_rr2 = (self.dma_rr2 + 1) % self.n_pool_sems
            else:
                k = self.dma_rr
                self.dma_rr = (k + 1) % (self.n_dma_sems - self.n_pool_sems)
            o.sem = k
            self.dma_cnt[k] += 1
            o.val = 16 * self.dma_cnt[k]
            o.prev_dma = self.dma_last[k]
            self.dma_last[k] = o
            o.sig = True
        for d in out:
            d.sig = True
        o.deps = out
        for t in reads:
            if not dma:
                t.rs = [r for r in t.rs if r.dma or r.eng != eng]
            t.rs.append(o)
        for t in writes:
            t.w = o
            t.rs = []
        self.ops[eng].append(o)
        return o

    def barrier(self):
        lasts = []
        for e in self.ENGS:
            for o in reversed(self.ops[e]):
                if not o.dma and o.fn is not None:
                    lasts.append(o)
                    break
        lasts += [d for d in self.dma_last if d is not None]
        for o in lasts:
            o.sig = True
        for e in self.ENGS:
            b = Op(e, None, False)
            b.deps = [d for d in lasts if d.dma or d.eng != e]
            self.ops[e].append(b)

    def dma(self, out, in_, reads=(), writes=(), eng="sp", **kw):
        def fn(e):
            return e.dma_start(out=out, in_=in_, **kw)
        return self.op(eng, fn, reads, writes, dma=True)

    def emit(self):
        nc = self.nc
        from contextlib import ExitStack
        with ExitStack() as es:
            esem = {}
            for e in ("pe", "act", "dve", "pool"):
                esem[e] = es.enter_context(nc.semaphore("s_" + e))
            dsem = [es.enter_context(nc.semaphore("s_dma%d" % i)) for i in range(self.n_dma_sems)]
            for e in ("pe", "act", "dve", "pool"):
                r = 0
                for o in self.ops[e]:
                    if o.dma or o.fn is None:
                        continue
                    if o.sig:
                        r += 1
                        o.rank = r
            print("ops per engine:", {e: len(self.ops[e]) for e in self.ENGS},
                  "signalling:", {e: sum(1 for o in self.ops[e] if o.sig and not o.dma and o.fn is not None) for e in self.ENGS},
                  "dma max val:", max(self.dma_cnt) * 16)
            block = es.enter_context(nc.Block())

            def run(ename):
                def body(eh):
                    wm_e = {}
                    wm_d = {}

                    def wait_for(d):
                        if d.dma:
                            if wm_d.get(d.sem, 0) >= d.val:
                                return
                            wm_d[d.sem] = d.val
                            eh.wait_ge(dsem[d.sem], d.val)
                        else:
                            if wm_e.get(d.eng, 0) >= d.rank:
                                return
                            wm_e[d.eng] = d.rank
                            eh.wait_ge(esem[d.eng], d.rank)

                    for o in self.ops[ename]:
                        for d in o.deps:
                            wait_for(d)
                        if o.dma and o.prev_dma is not None:
                            wait_for(o.prev_dma)
                        if o.fn is None:
                            continue
                        ins = o.fn(eh)
                        if o.dma:
                            ins.then_inc(dsem[o.sem], 16)
                        elif o.sig:
                            ins.then_inc(esem[ename], 1)
                    if ename == "sp":
                        for k in range(self.n_dma_sems):
                            if self.dma_last[k] is not None:
                                wait_for(self.dma_last[k])
                return body

            block.tensor(run("pe"))
            block.scalar(run("act"))
            block.vector(run("dve"))
            block.gpsimd(run("pool"))
            block.sync(run("sp"))


class SB:
    def __init__(self, nc, base=16512):
        self.nc = nc
        self.cur = base
        self.n = 0
        self.hi = base

    def t(self, shape, dt, name=None):
        sz = 1
        for s in shape[1:]:
            sz *= s
        sz *= 2 if dt == BF16 else 4
        sz = (sz + 31) // 32 * 32
        self.n += 1
        h = self.nc.alloc_sbuf_tensor_at("%s_%d_%d" % (name or "t", self.cur, self.n), list(shape), dt,
                                          offset=self.cur)
        self.cur += sz
        self.hi = max(self.hi, self.cur)
        assert self.cur <= 229376, ("SBUF overflow", self.cur)
        return h


def build(S, debug=None):
    NSUP = S // 512
    NT = S // 128
    NOWN = NSUP
    NCT = (S // 16 + 127) // 128
    debug = debug or {}
    nc = bass.Bass("TRN2", target_bir_lowering=False)
    P = Prog(nc)

    def din(name, shape, dt=F32):
        return nc.dram_tensor(name, list(shape), dt, kind="ExternalInput").ap()

    xfull = din("xfull", [S, D])
    xown = din("xown", [NOWN * 128, D])
    mem = din("mem", [256, D])
    w_in = din("w_in", [D, WEND])
    w1k = din("w1k", [2048, 256])
    w1v = din("w1v", [2048, 256])
    w2k = din("w2k", [256, 64])
    w2v = din("w2v", [256, 64])
    peT = din("peT", [128, 32])
    lbl_bc = din("lbl_bc", [128, 2 * 512])
    lbl_km = din("lbl_km", [128, 8])
    nsag_bc = din("nsag_bc", [128, 512])
    hgg_bc = din("hgg_bc", [128, 512])
    c_f32 = din("c_f32", [128, 128 * 3 + 2])
    c_msel = din("c_msel", [128, 33])
    c_ex32 = din("c_ex32", [128, 64 * 128])
    c_seg = din("c_seg", [128, 512])
    c_moe = din("c_moe", [128, 128 + 12 + 192])
    m_core = din("m_core", [128, 16 * 128])
    v_core = din("v_core", [128, 21])
    w_out = din("w_out", [D, D])
    wq = din("xa_wq", [D, D])
    wk = din("xa_wk", [D, D])
    wv = din("xa_wv", [D, D])
    wo = din("xa_wo", [D, D])
    ln_bc = din("ln_bc", [128, 6 * D])
    mo_wr = din("mo_wr", [D, 36])
    mo_br = din("mo_br", [128, 36])
    mo_wgu = din("mo_wgu", [32 * 4 * 128, 2 * 1024])
    mo_wd2 = din("mo_wd2", [32 * 2 * 128, 2 * 1024])
    out = nc.dram_tensor("out", [NOWN * 128, D], F32, kind="ExternalOutput").ap()
    mixT_d = nc.dram_tensor("mixT_d", [NOWN, 128, 8 * 128], BF16, kind="Internal").ap()
    x2_d = nc.dram_tensor("x2_d", [NOWN * 128, D], F32, kind="Internal").ap()
    dbg = {}
    for k, shp in debug.items():
        if not isinstance(shp, (list, tuple)):
            continue
        dbg[k] = nc.dram_tensor("dbg_" + k, list(shp), F32, kind="ExternalOutput").ap()

    PS = [nc.alloc_psum_tensor("ps%d" % i, [128, 512], F32) for i in range(8)]
    PSB = [nc.alloc_psum_tensor("psb%d" % i, [128, 1024], BF16) for i in range(0)]
    pst = [Tok("ps%d" % i, excl=True) for i in range(8)]

    def act(out_, in_, func, reads, writes, **kw):
        return P.op("act", lambda e: e.activation(out=out_, in_=in_, func=func, **kw), reads, writes)

    def mm(out_, lhsT, rhs, reads, writes, start=True, stop=True, **kw):
        return P.op("pe", lambda e: e.matmul(out_, lhsT, rhs, start=start, stop=stop, **kw), reads, writes)

    def tr(out_, in_, ident, reads, writes):
        return P.op("pe", lambda e: e.transpose(out_, in_, ident), reads, writes)

    def V(eng, name, reads, writes, *a, **kw):
        return P.op(eng, lambda e: getattr(e, name)(*a, **kw), reads, writes)

    sb = SB(nc)
    cf = sb.t([128, 386], F32, "cf"); t_cf = Tok()
    P.dma(cf[:, :], c_f32, (), (t_cf,))
    identf = cf[:, 0:128]; msuf = cf[:, 128:256]; chind = cf[:, 384:386]
    cb = sb.t([128, 128 * 2], BF16, "cb"); t_cb = Tok()
    P.dma(cb[:, 0:128], c_f32[:, 0:128], (), (t_cb,), eng="pool")
    P.dma(cb[:, 128:256], c_f32[:, 256:384], (), (t_cb,), eng="pool")
    identb = cb[:, 0:128]; trib = cb[:, 128:256]
    msel = sb.t([128, 33], BF16, "msel"); t_msel = Tok()
    P.dma(msel[:, :], c_msel, (), (t_msel,), eng="pool")
    ex32 = sb.t([128, 64, 128], BF16, "ex32"); t_ex = Tok()
    P.dma(ex32[:, :, :], c_ex32.rearrange("p (v k) -> p v k", v=64), (), (t_ex,), eng="pool")
    seg = sb.t([128, 512], F32, "seg"); t_seg = Tok()
    P.dma(seg[:, :], c_seg, (), (t_seg,))
    mcore = sb.t([128, 16, 128], BF16, "mcore"); t_mc = Tok()
    P.dma(mcore[:, :, :], m_core.rearrange("p (v k) -> p v k", v=16), (), (t_mc,), eng="pool")
    vcore = sb.t([128, 21], F32, "vcore"); t_vc = Tok()
    P.dma(vcore[:, :], v_core, (), (t_vc,))
    zrow = sb.t([128, 512], BF16, "zrow"); t_z = Tok()
    V("pool", "memset", (), (t_z,), zrow[:, :], 0.0)
    eps6 = sb.t([128, 2], F32, "eps6")
    V("pool", "memset", (), (t_z,), eps6[:, 0:1], 1e-6)
    V("pool", "memset", (), (t_z,), eps6[:, 1:2], 1e-5)
    onesb = sb.t([128, 2], BF16, "onesb")
    V("pool", "memset", (), (t_z,), onesb[:, :], 1.0)

    if debug.get("cut1"):
        P.emit()
        return nc
    W = sb.t([128, 8, WN], BF16, "W"); t_W = Tok()
    w_in_v = w_in.rearrange("(kt p) c -> p kt c", p=128)
    for kt in range(8):
        P.dma(W[:, kt, :], w_in_v[:, kt, WB:WEND], (), (t_W,), eng="pool")
    Wq = sb.t([128, 8, 4, 128], BF16, "Wq")
    for r in range(4):
        for g in range(2):
            h = g * 4 + r
            P.dma(Wq[:, :, r, g * 64:(g + 1) * 64], w_in_v[:, :, h * 64:(h + 1) * 64], (), (t_W,), eng="pool")
    Wcd = sb.t([128, 8, 4, 128], BF16, "Wcd")
    for idx, base in enumerate((KC0, KC0 + 64, VC0, VC0 + 64)):
        for dup in range(2):
            P.dma(Wcd[:, :, idx, dup * 64:(dup + 1) * 64], w_in_v[:, :, base:base + 64], (), (t_W,), eng="pool")
    W1 = sb.t([128, 2, 16, 256], BF16, "W1")
    P.dma(W1[:, 0, :, :], w1k.rearrange("(jp p) h -> p jp h", p=128), (), (t_W,), eng="pool")
    P.dma(W1[:, 1, :, :], w1v.rearrange("(jp p) h -> p jp h", p=128), (), (t_W,), eng="pool")
    W2p = sb.t([128, 2, 2, 128], BF16, "W2p")
    V("pool", "memset", (), (t_W,), W2p[:, :, :, :], 0.0)
    for ht in range(2):
        for g in range(2):
            P.dma(W2p[:, ht, g, g * 64:(g + 1) * 64], w2k[ht * 128:(ht + 1) * 128, :], (), (t_W,), eng="pool")
    W2pv = sb.t([128, 2, 2, 128], BF16, "W2pv")
    V("pool", "memset", (), (t_W,), W2pv[:, :, :, :], 0.0)
    for ht in range(2):
        for g in range(2):
            P.dma(W2pv[:, ht, g, g * 64:(g + 1) * 64], w2v[ht * 128:(ht + 1) * 128, :], (), (t_W,), eng="pool")
    VCT = sb.t([128, NCT * 128], BF16, "VCT")
    pet = sb.t([128, 32], BF16, "pet")
    P.dma(pet[:, :], peT, (), (t_W,), eng="pool")
    nsag = sb.t([128, 512], F32, "nsag"); hgg = sb.t([128, 512], F32, "hgg")
    P.dma(nsag[:, :], nsag_bc, (), (t_W,))
    P.dma(hgg[:, :], hgg_bc, (), (t_W,))
    if debug.get("cut2"):
        P.emit()
        return nc
    hh = sb.t([128, 3, 512], F32, "hh")
    lbt = hh[:, 0:2, :].rearrange("p a b -> p (a b)"); t_lb = Tok()
    P.dma(lbt, lbl_bc, (), (t_lb,))
    lbk = sb.t([128, 8], F32, "lbk")
    P.dma(lbk[:, :], lbl_km, (), (t_lb,))
    lb_bc = sb.t([128, 512], F32, "lb_bc"); oml_bc = sb.t([128, 512], F32, "oml_bc")
    lb_k = sb.t([128, 4], F32, "lb_k"); oml_k = sb.t([128, 4], F32, "oml_k")
    for (src0, src1, lbo, omlo) in ((lbt[:, 0:512], lbt[:, 512:1024], lb_bc[:, :], oml_bc[:, :]),
                                     (lbk[:, 0:4], lbk[:, 4:8], lb_k[:, :], oml_k[:, :])):
        V("dve", "tensor_sub", (t_lb,), (t_lb,), omlo, src1, src0)
        act(omlo, omlo, AF.Exp, (t_lb,), (t_lb,))
        V("dve", "tensor_scalar_add", (t_lb,), (t_lb,), omlo, omlo, 1.0)
        V("dve", "reciprocal", (t_lb,), (t_lb,), lbo, omlo)
        V("dve", "tensor_scalar", (t_lb,), (t_lb,), omlo, lbo, -1.0, 1.0, ALU.mult, ALU.add)

    if debug.get("cut3"):
        P.emit()
        return nc
    biasc = sb.t([128, 4], F32, "biasc"); t_bias = Tok()
    for kv in range(2):
        for ht in range(2):
            for jp in range(16):
                mm(PS[0][:, kv * 2 + ht:kv * 2 + ht + 1], W1[:, kv, jp, ht * 128:(ht + 1) * 128],
                   pet[:, kv * 16 + jp:kv * 16 + jp + 1], (t_W,), (pst[0],), start=(jp == 0), stop=(jp == 15))
    V("dve", "tensor_copy", (pst[0],), (t_bias,), biasc[:, :], PS[0][:, 0:4])

    if debug.get("cut4"):
        P.emit()
        return nc
    ksT_d = nc.dram_tensor("ksT_d", [128, S], BF16, kind="Internal").ap()
    vsA_d = nc.dram_tensor("vsA_d", [128, NT * 130], BF16, kind="Internal").ap()
    kvst = [sb.t([128, 128 + 130], BF16, "kvst") for _ in range(2)]; t_kvst = [Tok(), Tok()]
    NRING = 4
    kring = [sb.t([128, 512 + 4 * 130], BF16, "kring") for _ in range(NRING)]; t_ring = [Tok() for _ in range(NRING)]
    kwR = sb.t([128, 16, 128], BF16, "kwR")
    vwR = sb.t([128, 16, 2, 65], BF16, "vwR")
    KCT = sb.t([128, NCT * 128], BF16, "KCT")
    VCA = sb.t([128, NCT, 2, 65], BF16, "VCA")
    t_ks = [Tok() for _ in range(NT)]
    t_kw = [Tok() for _ in range(16)]
    t_kc = [Tok() for _ in range(NCT)]
    t_init = Tok()
    V("pool", "memset", (), (t_init,), KCT[:, :], 0.0)
    V("pool", "memset", (), (t_init,), VCT[:, :], 0.0)
    V("pool", "memset", (), (t_init,), VCA[:, :, :, :], 0.0)
    for q_ in range(2):
        V("pool", "memset", (), (t_kvst[q_],), kvst[q_][:, 128:258], 1.0)
    V("pool", "memset", (), (t_init,), vwR[:, :, :, 64:65], 1.0)
    for ct in range(NCT):
        V("pool", "memset", (t_init,), (t_kc[ct],), VCA[:, ct, :, 64:65], 1.0)
    for j in range(16):
        t_kw[j].w = t_init.w
    KKW = 16 + 512 + 16
    KK = sb.t([128, 2, 4, KKW], BF16, "KK")
    t_kk = [Tok(), Tok()]
    V("pool", "memset", (), (t_kk[0], t_kk[1]), KK[:, :, :, :], 0.0)

    Sst = sb.t([128, 512], F32, "Sst"); t_S = Tok()
    V("pool", "memset", (), (t_S,), Sst[:, :], 0.0)
    Sown = sb.t([128, 2, 2, 512], BF16, "Sown")
    t_so = [Tok(), Tok()]

    if debug.get("cut5"):
        P.emit()
        return nc
    NXB = 2
    xs = [sb.t([128, D], F32, "xs") for _ in range(NXB)]; t_xs = [Tok() for _ in range(NXB)]
    xT = [sb.t([128, 8, 128], BF16, "xT") for _ in range(2)]; t_xT = [Tok(), Tok()]
    xTo = sb.t([128, 8, 128], BF16, "xTo"); t_xTo = Tok()
    vsb = sb.t([128, 512], BF16, "vsb"); t_vsb = Tok()
    h1 = hh[:, 0, :]; h2 = hh[:, 1, :]; h3 = hh[:, 2, :]
    t_h1, t_h2, t_h3 = Tok(), Tok(), Tok()
    t_h1.w = t_lb.w; t_h2.w = t_lb.w; t_h1.rs = t_lb.rs; t_h2.rs = t_lb.rs
    kkb = sb.t([128, 2, 512], BF16, "kkb"); t_kkb = Tok()
    V("pool", "memset", (), (t_kkb,), kkb[:, :, :], 0.0)
    dec = sb.t([128, 8], F32, "dec"); t_dec = Tok()
    blt = sb.t([128, 512], BF16, "blt"); t_bl = Tok()

    def load_xT(src_ap, k, XT, tXT, banks=(0, 1), q="sp"):
        b = k % NXB
        P.dma(xs[b][:, :], src_ap, (), (t_xs[b],), eng=q)
        for half in range(2):
            bk = banks[half]
            bank = PS[bk]
            for q in range(4):
                kt = half * 4 + q
                tr(bank[:, q * 128:(q + 1) * 128], xs[b][:, kt * 128:(kt + 1) * 128], identf,
                   (t_xs[b], t_cf), (pst[bk],))
            if half == 0:
                act(XT[:, 0:4, :], bank[:, :].rearrange("p (a b) -> p a b", a=4), AF.Copy,
                    (pst[bk],), (tXT,))
            else:
                V("dve", "tensor_copy", (pst[bk],), (tXT,), XT[:, 4:8, :],
                  bank[:, :].rearrange("p (a b) -> p a b", a=4))

    def full_tile(j, kidx):
        p = kidx % 2
        load_xT(xfull[j * 128:(j + 1) * 128, :], kidx, xT[p], t_xT[p], banks=(2, 4), q="pool")
        X = xT[p]; tX = t_xT[p]
        yield
        sup = j // 4
        par = sup % 2
        for n_, off in enumerate((oKS, oKW)):
            for kt in range(8):
                mm(PS[5][:, n_ * 128:(n_ + 1) * 128], W[:, kt, off:off + 128], X[:, kt, :],
                   (t_W, tX), (pst[5],), start=(kt == 0), stop=(kt == 7))
        slot = j % 16
        sq = j % 2
        act(kvst[sq][:, 0:128], PS[5][:, 0:128], AF.Copy, (pst[5],), (t_kvst[sq],))
        V("dve", "tensor_copy", (pst[5],), (t_kw[slot],), kwR[:, slot, :], PS[5][:, 128:256])
        yield
        for idx in range(4):
            for kt in range(8):
                mm(PS[2][:, idx * 128:(idx + 1) * 128], Wcd[:, kt, idx, :], X[:, kt, :],
                   (t_W, tX), (pst[2],), start=(kt == 0), stop=(kt == 7))
        lo = 16 + (j % 4) * 128
        act(KK[0:64, par, :, lo:lo + 128], PS[2][0:64, :].rearrange("p (a b) -> p a b", a=4), AF.Copy,
            (pst[2],), (t_kk[par],))
        V("dve", "tensor_copy", (pst[2],), (t_kk[par],), KK[64:128, par, :, lo - 1:lo + 127],
          PS[2][64:128, :].rearrange("p (a b) -> p a b", a=4))
        if j % 4 == 0 and j > 0:
            V("pool", "tensor_copy", (t_kk[par],), (t_kk[1 - par],), KK[0:64, 1 - par, :, 528:544],
              KK[0:64, par, :, 16:32])
            V("pool", "tensor_copy", (t_kk[par],), (t_kk[1 - par],), KK[64:128, 1 - par, :, 527:544],
              KK[64:128, par, :, 15:32])
        yield
        for n_, off in enumerate((oVS, oVW)):
            for kt in range(8):
                mm(PS[4][:, n_ * 128:(n_ + 1) * 128], X[:, kt, :], W[:, kt, off:off + 128],
                   (t_W, tX), (pst[4],), start=(kt == 0), stop=(kt == 7))
        act(kvst[sq][:, 128:258].rearrange("p (g d) -> p g d", g=2)[:, :, 0:64],
            PS[4][:, 0:128].rearrange("p (g d) -> p g d", g=2), AF.Copy, (pst[4],), (t_kvst[sq],))
        P.dma(ksT_d[:, j * 128:(j + 1) * 128], kvst[sq][:, 0:128], (t_kvst[sq],), (t_ks[j],), eng="act")
        P.dma(vsA_d[:, j * 130:(j + 1) * 130], kvst[sq][:, 128:258], (t_kvst[sq],), (t_ks[j],), eng="act")
        V("dve", "tensor_copy", (pst[4],), (t_kw[slot],), vwR[:, slot, :, 0:64],
          PS[4][:, 128:256].rearrange("p (g d) -> p g d", g=2))
        yield
        for bank, off in ((5, oHF), (2, oHI)):
            for kt in range(8):
                mm(PS[bank][:, :], X[:, kt, :], W[:, kt, off:off + 512], (t_W, tX), (pst[bank],),
                   start=(kt == 0), stop=(kt == 7))
            yield
        act(vsb[:, :], PS[2][:, :], AF.Copy, (pst[2],), (t_vsb,))
        act(h1[:, :], PS[5][:, :], AF.Exp, (pst[5],), (t_h1,), scale=-1.0)
        V("dve", "tensor_scalar_add", (t_h1,), (t_h1,), h1[:, :], h1[:, :], 1.0); V("dve", "reciprocal", (t_h1,), (t_h1,), h1[:, :], h1[:, :])
        V("dve", "tensor_mul", (t_h1, t_lb), (t_h1,), h1[:, :], h1[:, :], oml_bc[:, :])
        V("dve", "tensor_add", (t_h1, t_lb), (t_h1,), h1[:, :], h1[:, :], lb_bc[:, :])
        act(h2[:, :], h1[:, :], AF.Ln, (t_h1,), (t_h2,))
        V("dve", "tensor_scalar", (t_h1,), (t_h1,), h1[:, :], h1[:, :], -1.0, 1.0, ALU.mult, ALU.add)
        yield
        mm(PS[4][:, :], msuf, h2[:, :], (t_cf, t_h2), (pst[4],))
        act(h3[:, :], PS[4][:, :], AF.Exp, (pst[4],), (t_h3,))
        for c in range(2):
            V("dve", "tensor_mul", (t_h1, t_h3), (t_kkb,), kkb[64 * c:64 * c + 64, c, :], h1[64 * c:64 * c + 64, :],
              h3[64 * c:64 * c + 64, :])
        for h in range(4):
            mm(PS[5][:, 256 + 2 * h:256 + 2 * h + 2], h2[:, h * 128:(h + 1) * 128], chind, (t_h2, t_cf),
               (pst[5],))
        act(dec[:, :], PS[5][:, 256:264], AF.Exp, (pst[5],), (t_dec,))
        yield
        for c in range(2):
            jj = j % 4
            if jj == 0:
                V("dve", "tensor_scalar_mul", (t_S, t_vc), (t_so[par],), Sown[:, par, c, :], Sst[:, :],
                  vcore[:, 17:18])
            else:
                V("dve", "scalar_tensor_tensor", (t_S, t_vc, t_so[par]), (t_so[par],), Sown[:, par, c, :],
                  Sst[:, :], vcore[:, 17 + jj:18 + jj], Sown[:, par, c, :], ALU.mult, ALU.add)
            for h in range(4):
                mm(PS[4][:, h * 128:(h + 1) * 128], kkb[:, c, h * 128:(h + 1) * 128],
                   vsb[:, h * 128:(h + 1) * 128], (t_kkb, t_vsb), (pst[4],))
            for h in range(4):
                V("dve", "scalar_tensor_tensor", (t_S, t_dec, pst[4]), (t_S,), Sst[:, h * 128:(h + 1) * 128],
                  Sst[:, h * 128:(h + 1) * 128], dec[:, 2 * h + c:2 * h + c + 1],
                  PS[4][:, h * 128:(h + 1) * 128], ALU.mult, ALU.add)
            yield

    u_sb = sb.t([128, 8, 32], F32, "u_sb"); t_u = Tok()
    t_vct = Tok()
    PSb1 = PS[1][:, :].bitcast(BF16)
    u2 = sb.t([128, 256], F32, "u2"); t_u2 = Tok()
    gl = sb.t([128, 8, 32], BF16, "gl"); t_gl = Tok()

    def compress(i):
        par = i % 2
        for kv in range(2):
            for g in range(2):
                idx = kv * 2 + g
                for ht in range(2):
                    col = (idx * 2 + ht) * 32
                    for jp in range(16):
                        rhs = KK[:, par, idx, 16 + 2 * jp:16 + 2 * jp + 497:16]
                        mm(PS[0][:, col:col + 32], W1[:, kv, jp, ht * 128:(ht + 1) * 128], rhs,
                           (t_W, t_kk[par]), (pst[0],), start=(jp == 0), stop=(jp == 15))
        for kv in range(2):
            for ht in range(2):
                for g in range(2):
                    s_ = ((kv * 2 + g) * 2 + ht)
                    act(u_sb[:, s_, :], PS[0][:, s_ * 32:(s_ + 1) * 32], AF.Identity, (pst[0], t_bias), (t_u,),
                        bias=biasc[:, kv * 2 + ht:kv * 2 + ht + 1])
        uf = u_sb[:, :, :].rearrange("p a b -> p (a b)")
        V("dve", "tensor_mul", (t_u,), (t_u2,), u2[:, :], uf, uf)
        V("dve", "tensor_scalar", (t_u2,), (t_u2,), u2[:, :], u2[:, :], 0.044715, 1.0, ALU.mult, ALU.add)
        V("dve", "tensor_mul", (t_u2, t_u), (t_u2,), u2[:, :], u2[:, :], uf)
        act(u2[:, :], u2[:, :], AF.Exp, (t_u2,), (t_u2,), scale=-1.5957691216057308)
        V("dve", "tensor_scalar_add", (t_u2,), (t_u2,), u2[:, :], u2[:, :], 1.0); V("dve", "reciprocal", (t_u2,), (t_u2,), u2[:, :], u2[:, :])
        V("dve", "tensor_mul", (t_u2, t_u), (t_gl,), gl[:, :, :].rearrange("p a b -> p (a b)"), u2[:, :], uf)
        ct = i // 4
        q = i % 4
        first = True
        for g in range(2):
            for ht in range(2):
                s_ = ((0 * 2 + g) * 2 + ht)
                mm(PS[1][:, 0:32], W2p[:, ht, g, :], gl[:, s_, :], (t_W, t_gl), (pst[1],), start=first,
                   stop=(g == 1 and ht == 1))
                first = False
        act(KCT[:, i * 32:(i + 1) * 32], PS[1][:, 0:32], AF.Copy, (pst[1],), (t_kc[ct],))
        first = True
        for g in range(2):
            for ht in range(2):
                s_ = ((1 * 2 + g) * 2 + ht)
                mm(PS[1][:, 64:96], W2pv[:, ht, g, :], gl[:, s_, :], (t_W, t_gl), (pst[1],), start=first,
                   stop=(g == 1 and ht == 1))
                first = False
        V("dve", "tensor_copy", (pst[1],), (t_vct,), VCT[:, i * 32:(i + 1) * 32], PS[1][:, 64:96])
        tr(PSb1[:, 256:384], VCT[:, ct * 128:(ct + 1) * 128], identb, (t_vct, t_cb), (pst[1],))
        V("dve", "tensor_copy", (pst[1],), (t_kc[ct],), VCA[:, ct, :, 0:64],
          PSb1[:, 256:384].rearrange("p (g d) -> p g d", g=2))

    qT = sb.t([128, 2, 4, 128], BF16, "qT"); t_qT = Tok()
    V("pool", "memset", (), (t_qT,), qT[:, :, :, :], 0.0)
    gts = sb.t([128, 24], F32, "gts"); t_g = Tok()
    Pt = [sb.t([128, 512], BF16, "Pt") for _ in range(4)]; t_Pt = [Tok() for _ in range(4)]
    sc = sb.t([128, 256], F32, "sc"); sc2 = sb.t([128, 256], F32, "sc2"); t_sc = Tok()
    m8 = sb.t([128, 16], F32, "m8")
    nsel = sb.t([128, 256], BF16, "nsel"); t_nsel = Tok()
    V("pool", "memset", (), (t_nsel,), nsel[:, :], 0.0)
    nselT = sb.t([128, 2, 2, 4, 128], BF16, "nselT"); t_nselT = [Tok(), Tok()]
    V("pool", "memset", (), (t_nselT[0], t_nselT[1]), nselT[:, :, :, :, :], 0.0)
    rden = sb.t([128, 24], F32, "rden"); t_rd = Tok()
    grd = sb.t([128, 24], F32, "grd"); t_grd = Tok()
    onsa = sb.t([128, 512], F32, "onsa"); t_on = Tok()
    mixn = sb.t([128, 1024], BF16, "mixn"); t_mixn = Tok()
    mixT = [sb.t([128, 8, 128], BF16, "mixT") for _ in range(1)] * 2; t_mixT = [Tok()] * 2
    st6 = sb.t([128, 8, 6], F32, "st6"); mv = sb.t([128, 8, 2], F32, "mv"); t_st = Tok()
    g1 = sb.t([128, 512], F32, "g1"); g2 = sb.t([128, 512], F32, "g2"); t_g1, t_g2 = Tok(), Tok()
    qdT = sb.t([128, 512], BF16, "qdT"); kdT = sb.t([128, 512], BF16, "kdT"); qsT = sb.t([128, 2, 512], BF16, "qsT")
    t_qd, t_kd, t_qs = Tok(), Tok(), Tok()
    V("pool", "memset", (), (t_qs,), qsT[:, :, :], 0.0)
    attm = sb.t([128, 512], BF16, "attm"); t_attm = Tok()
    vob = sb.t([128, 512], BF16, "vob"); t_vob = Tok()
    sgt = sb.t([128, 512], F32, "sgt"); t_sgt = Tok()
    print("phase A SBUF bytes/partition:", sb.cur)
    pcnt = [0]
    rcnt = [0]
    ptc = [0]

    def own_tile(i, kidx, bg=None):
        load_xT(xown[i * 128:(i + 1) * 128, :], kidx, xTo, t_xTo)
        X = xTo; tX = t_xTo
        par = i % 2
        for r in range(4):
            for kt in range(8):
                mm(PS[2][:, r * 128:(r + 1) * 128], Wq[:, kt, r, :], X[:, kt, :], (t_W, tX), (pst[2],),
                   start=(kt == 0), stop=(kt == 7))
        act(qT[0:64, 0, :, :], PS[2][0:64, :].rearrange("p (a b) -> p a b", a=4), AF.Copy, (pst[2],), (t_qT,), scale=0.125)
        act(qT[64:128, 1, :, :], PS[2][64:128, :].rearrange("p (a b) -> p a b", a=4), AF.Copy, (pst[2],), (t_qT,),
            scale=0.125)
        for kt in range(8):
            mm(PS[3][:, 0:24], X[:, kt, :], W[:, kt, oG:oG + 24], (t_W, tX), (pst[3],), start=(kt == 0),
               stop=(kt == 7))
        act(gts[:, :], PS[3][:, 0:24], AF.Exp, (pst[3],), (t_g,), scale=-1.0)
        V("dve", "tensor_scalar_add", (t_g,), (t_g,), gts[:, :], gts[:, :], 1.0); V("dve", "reciprocal", (t_g,), (t_g,), gts[:, :], gts[:, :])

        def zinit(bank, lo, hi):
            mm(PS[bank][:, lo:hi], zrow[:, 0:128], zrow[:, 0:hi - lo], (t_z,), (pst[bank],), start=True, stop=False,
               skip_group_check=True)

        def s_stage(it):
            g = it["g"]
            masks = it["masks"]
            banks = it.get("sbanks", (0, 1))
            sbk = banks[pcnt[0] % len(banks)]
            pcnt[0] += 1
            sps = PS[sbk]
            allwide = all(m[3] for m in masks)
            if allwide:
                mm(sps[:, :], it["kT"], qT[:, g, :, :].rearrange("p a b -> p (a b)"),
                   tuple(it["rds"]) + (t_qT,), (pst[sbk],), start=True, stop=(len(masks) == 0))
                for mi, (ml, mr, mrd, wide) in enumerate(masks):
                    mm(sps[:, :], ml, mr, tuple(mrd), (pst[sbk],), start=False, stop=(mi == len(masks) - 1))
            else:
                for r in range(4):
                    mm(sps[:, r * 128:(r + 1) * 128], it["kT"], qT[:, g, r, :],
                       tuple(it["rds"]) + (t_qT,), (pst[sbk],), start=True, stop=False)
                    for mi, (ml, mr, mrd, wide) in enumerate(masks):
                        mr_ = mr[:, r * 128:(r + 1) * 128] if wide else mr
                        mm(sps[:, r * 128:(r + 1) * 128], ml, mr_, tuple(mrd), (pst[sbk],), start=False,
                           stop=(mi == len(masks) - 1))
            pb = ptc[0] % 4
            ptc[0] += 1
            act(Pt[pb][:, :], sps[:, :], AF.Exp, (pst[sbk],), (t_Pt[pb],))
            return pb

        def pv_stage(it, pb):
            acc = PS[it["acc"]]
            pt_ap = Pt[pb][:, :]; tp = t_Pt[pb]
            for r in range(4):
                mm(acc[:, r * 65:(r + 1) * 65], pt_ap[:, r * 128:(r + 1) * 128], it["vA"], (tp,) + tuple(it["rds"]),
                   (pst[it["acc"]],), start=False, stop=it["last"], skip_group_check=True)
            if it.get("imp_ct") is not None:
                ct = it["imp_ct"]
                wcols = min(33, 256 - 32 * ct)
                for r in range(4):
                    bank = 3 + r // 2
                    base = (r % 2) * 256 + 32 * ct
                    mm(PS[bank][:, base:base + wcols], pt_ap[:, r * 128:(r + 1) * 128], msel[:, 0:wcols],
                       (tp, t_msel), (pst[bank],), start=False, stop=it["last"], skip_group_check=True)

        def run_pipeline(items, depth=1, prep=None, bgstep=None):
            pend = []
            for it in items:
                if it.get("pre") is not None:
                    it["pre"]()
                if prep is not None:
                    prep(it)
                pb = s_stage(it)
                pend.append((it, pb))
                if len(pend) > depth:
                    pv_stage(*pend.pop(0))
                if bgstep is not None:
                    bgstep()
            for pr in pend:
                pv_stage(*pr)

        def finish(g, accbank, br):
            acc = PS[accbank]
            for r in range(4):
                c_ = (g * 4 + r) * 3 + br
                V("dve", "tensor_scalar_max", (pst[accbank],), (t_rd,), rden[:, c_:c_ + 1],
                  acc[:, r * 65 + 64:r * 65 + 65], 1e-30)
                V("dve", "reciprocal", (t_rd,), (t_rd,), rden[:, c_:c_ + 1], rden[:, c_:c_ + 1])
            sl = slice(g * 12 + br, g * 12 + 12, 3)
            V("dve", "tensor_mul", (t_rd, t_g), (t_grd,), grd[:, sl], rden[:, sl], gts[:, sl])

        def gate_out(g, accbank, br):
            acc = PS[accbank]
            for r in range(4):
                h = g * 4 + r
                c_ = h * 3 + br
                if br == 0:
                    V("dve", "tensor_scalar_mul", (pst[accbank], t_grd), (t_on,), onsa[:, h * 64:(h + 1) * 64],
                      acc[:, r * 65:r * 65 + 64], grd[:, c_:c_ + 1])
                else:
                    V("dve", "scalar_tensor_tensor", (pst[accbank], t_grd, t_on), (t_on,),
                      onsa[:, h * 64:(h + 1) * 64], acc[:, r * 65:r * 65 + 64], grd[:, c_:c_ + 1],
                      onsa[:, h * 64:(h + 1) * 64], ALU.mult, ALU.add)

        for n_, off in enumerate((oHQ, oHF)):
            for h in range(4):
                for kt in range(8):
                    mm(PS[2 + n_][:, h * 128:(h + 1) * 128], W[:, kt, off + h * 128:off + (h + 1) * 128], X[:, kt, :],
                       (t_W, tX), (pst[2 + n_],), start=(kt == 0), stop=(kt == 7))
        for bank, off in ((4, oHI), (5, oHG)):
            for kt in range(8):
                mm(PS[bank][:, :], X[:, kt, :], W[:, kt, off:off + 512], (t_W, tX), (pst[bank],), start=(kt == 0),
                   stop=(kt == 7))
        act(vob[:, :], PS[4][:, :], AF.Copy, (pst[4],), (t_vob,))
        act(sgt[:, :], PS[5][:, :], AF.Exp, (pst[5],), (t_sgt,), scale=-1.0)
        V("dve", "tensor_scalar_add", (t_sgt,), (t_sgt,), sgt[:, :], sgt[:, :], 1.0); V("dve", "reciprocal", (t_sgt,), (t_sgt,), sgt[:, :], sgt[:, :])
        V("dve", "tensor_mul", (t_sgt, pst[5]), (t_sgt,), sgt[:, :], sgt[:, :], PS[5][:, :])
        V("dve", "tensor_mul", (t_sgt, t_W), (t_sgt,), sgt[:, :], sgt[:, :], hgg[:, :])
        act(g1[:, :], PS[3][:, :], AF.Exp, (pst[3],), (t_g1,), scale=-1.0)
        V("dve", "tensor_scalar_add", (t_g1,), (t_g1,), g1[:, :], g1[:, :], 1.0); V("dve", "reciprocal", (t_g1,), (t_g1,), g1[:, :], g1[:, :])
        for h in range(4):
            V("dve", "tensor_scalar", (t_g1, t_lb), (t_g1,), g1[:, h * 128:(h + 1) * 128], g1[:, h * 128:(h + 1) * 128],
              oml_k[:, h:h + 1], lb_k[:, h:h + 1], ALU.mult, ALU.add)
        act(g2[:, :], g1[:, :], AF.Ln, (t_g1,), (t_g2,))
        V("dve", "tensor_scalar", (t_g1,), (t_g1,), g1[:, :], g1[:, :], -1.0, 1.0, ALU.mult, ALU.add)
        V("dve", "tensor_tensor_scan", (t_seg, t_g2), (t_h1,), h1[:, :], seg[:, :], g2[:, :], 0.0, ALU.mult, ALU.add)
        act(h2[:, :], h1[:, :], AF.Exp, (t_h1,), (t_h2,))
        for c in range(2):
            V("dve", "tensor_mul", (t_h2, pst[2]), (t_qs,),
              qsT[:, c, :].rearrange("p (h t) -> p h t", h=4)[:, :, 64 * c:64 * c + 64],
              h2[:, :].rearrange("p (h t) -> p h t", h=4)[:, :, 64 * c:64 * c + 64],
              PS[2][:, :].rearrange("p (h t) -> p h t", h=4)[:, :, 64 * c:64 * c + 64])
        for hc in range(8):
            V("dve", "tensor_scalar", (t_h1,), (t_h3,), h3[:, hc * 64:(hc + 1) * 64], h1[:, hc * 64:(hc + 1) * 64],
              h1[:, hc * 64 + 31:hc * 64 + 32], None, ALU.subtract)
        act(h2[:, :], h3[:, :], AF.Exp, (t_h3,), (t_h2,))
        V("dve", "tensor_mul", (t_h2, pst[2]), (t_qd,), qdT[:, :], h2[:, :], PS[2][:, :])
        act(g2[:, :], h3[:, :], AF.Exp, (t_h3,), (t_g2,), scale=-1.0)
        V("dve", "tensor_mul", (t_g2, t_g1), (t_kd,), kdT[:, :], g2[:, :], g1[:, :])
        for h in range(4):
            mm(PS[3][:, h * 128:(h + 1) * 128], kdT[:, h * 128:(h + 1) * 128], qdT[:, h * 128:(h + 1) * 128],
               (t_kd, t_qd), (pst[3],))
        V("dve", "tensor_mul", (pst[3], t_cb), (t_attm,), attm[:, :].rearrange("p (a b) -> p a b", a=4),
          PS[3][:, :].rearrange("p (a b) -> p a b", a=4), trib.unsqueeze(1).broadcast_to([128, 4, 128]))
        mm(PS[4][:, :], zrow[:, 0:128], zrow[:, :], (t_z,), (pst[4],), start=True, stop=False, skip_group_check=True)
        for h in range(4):
            hs = slice(h * 128, (h + 1) * 128)
            mm(PS[4][:, hs], attm[:, hs], vob[:, hs], (t_attm, t_vob), (pst[4],), start=False, stop=False,
               skip_group_check=True)
            for c in range(2):
                mm(PS[4][:, hs], qsT[:, c, hs], Sown[:, par, c, hs],
                   (t_qs, t_so[par]), (pst[4],), start=False, stop=True, skip_group_check=True)
        if debug.get("ohg") is not None:
            V("dve", "tensor_copy", (pst[4],), (t_g1,), g1[:, :], PS[4][:, :])
            P.dma(dbg["ohg"][i * 128:(i + 1) * 128, :], g1[:, :], (t_g1,), ())
        for h in range(4):
            V("dve", "bn_stats", (pst[4],), (t_st,), st6[:, 1 + h, :], PS[4][:, h * 128:(h + 1) * 128])
            V("dve", "bn_aggr", (t_st,), (t_st,), mv[:, 2 + h, :], st6[:, 1 + h, :])
            V("dve", "tensor_mul", (t_st,), (t_st,), st6[:, 1 + h, 0:1], mv[:, 2 + h, 0:1], mv[:, 2 + h, 0:1])
            V("dve", "tensor_add", (t_st,), (t_st,), st6[:, 1 + h, 0:1], st6[:, 1 + h, 0:1], mv[:, 2 + h, 1:2])
            act(st6[:, 1 + h, 1:2], st6[:, 1 + h, 0:1], AF.Ln, (t_st,), (t_st,), bias=eps6[:, 0:1])
            act(st6[:, 1 + h, 1:2], st6[:, 1 + h, 1:2], AF.Exp, (t_st,), (t_st,), scale=-0.5)
            V("dve", "scalar_tensor_tensor", (pst[4], t_st, t_sgt), (t_mixn,), mixn[:, 512 + h * 128:512 + (h + 1) * 128],
              PS[4][:, h * 128:(h + 1) * 128], st6[:, 1 + h, 1:2], sgt[:, h * 128:(h + 1) * 128], ALU.mult, ALU.mult)
        nct = i // 4 + 1
        ncols = 8 * i + 8
        njt = (ncols + 127) // 128
        for g in range(2):
            gs = slice(64 * g, 64 * g + 64)
            zinit(2, 0, 260)
            zinit(3, 0, 512)
            zinit(4, 0, 512)
            items = []
            for ct in range(nct):
                masks = []
                if ct == nct - 1:
                    masks = [(identb, mcore[:, 12 + (i % 4), :], (t_cb, t_mc), False)]
                items.append(dict(g=g, kT=KCT[:, ct * 128:(ct + 1) * 128], vA=VCA[:, ct, g, :], masks=masks,
                                  rds=(t_kc[ct],), acc=2, last=(ct == nct - 1), imp_ct=ct))
            run_pipeline(items)
            finish(g, 2, 0)
            gate_out(g, 2, 0)
            for r in range(4):
                bank = 3 + r // 2
                base = (r % 2) * 256
                c_ = (g * 4 + r) * 3 + 0
                if r == 0:
                    V("dve", "tensor_scalar_mul", (pst[bank], t_rd), (t_sc,), sc[:, 0:ncols],
                      PS[bank][:, base:base + ncols], rden[:, c_:c_ + 1])
                else:
                    V("dve", "scalar_tensor_tensor", (pst[bank], t_rd, t_sc), (t_sc,), sc[:, 0:ncols],
                      PS[bank][:, base:base + ncols], rden[:, c_:c_ + 1], sc[:, 0:ncols], ALU.mult, ALU.add)
            if i == 0:
                V("dve", "tensor_add", (t_sc, t_vc), (t_sc,), sc[:, 0:8], sc[:, 0:8], vcore[:, 9:17])
            else:
                V("dve", "tensor_add", (t_sc, t_vc), (t_sc,), sc[:, ncols - 9:ncols], sc[:, ncols - 9:ncols],
                  vcore[:, 0:9])
                V("dve", "tensor_scalar_add", (t_sc,), (t_sc,), sc[:, 0:1], sc[:, 0:1], 1.0e4)
            V("dve", "max", (t_sc,), (t_sc,), m8[:, 0:8], sc[:, 0:ncols])
            V("dve", "match_replace", (t_sc,), (t_sc,), sc2[:, 0:ncols], m8[:, 0:8], sc[:, 0:ncols], -1.0e30)
            V("dve", "max", (t_sc,), (t_sc,), m8[:, 0:8], sc2[:, 0:ncols])
            V("dve", "tensor_scalar", (t_sc,), (t_nsel,), nsel[:, 0:ncols], sc[:, 0:ncols], m8[:, 7:8], NEG,
              ALU.is_lt, ALU.mult)
            if debug.get("nsel") is not None and i == debug.get("_i", 0):
                V("dve", "tensor_copy", (t_nsel,), (t_sc,), sc2[:, 0:ncols], nsel[:, 0:ncols])
                P.dma(dbg["nsel"][g * 128:(g + 1) * 128, 0:ncols], sc2[:, 0:ncols], (t_sc,), ())
            for jt in range(njt):
                w_ = 128
                tr(PSb5[0:w_, jt * 128:(jt + 1) * 128], nsel[:, jt * 128:jt * 128 + w_], identb, (t_nsel, t_cb),
                   (pst[5],))
                V("dve", "tensor_copy", (pst[5],), (t_nselT[g],), nselT[0:w_, g, jt, :, :],
                  PSb5[0:w_, jt * 128:(jt + 1) * 128].unsqueeze(1).broadcast_to([w_, 4, 128]))
        zinit(6, 0, 260)
        zinit(7, 0, 260)
        slots = {}

        def fetch(qd):
            rs_ = rcnt[0] % NRING
            rcnt[0] += 1
            slots[qd] = rs_
            P.dma(kring[rs_][:, 0:512], ksT_d[:, qd * 512:(qd + 1) * 512], tuple(t_ks[4 * qd:4 * qd + 4]), (t_ring[rs_],))
            P.dma(kring[rs_][:, 512:1032], vsA_d[:, qd * 520:(qd + 1) * 520], tuple(t_ks[4 * qd:4 * qd + 4]),
                  (t_ring[rs_],))

        for qd in range(min(2, i + 1)):
            fetch(qd)
        items = []
        for qd in range(i + 1):
            for g in range(2):
                gs = slice(64 * g, 64 * g + 64)
                for kk in range(4):
                    kt = 4 * qd + kk
                    j0 = 2 * kt
                    jt, v_ = j0 // 128, (j0 % 128) // 2
                    it = dict(g=g, acc=6 + g, last=(qd == i and kk == 3), qd=qd, kk=kk, gs=gs)
                    it["mk"] = [(ex32[:, v_, :], nselT[:, g, jt, :, :].rearrange("p a b -> p (a b)"), (t_ex, t_nselT[g]), True)]
                    if kt >= 4 * i:
                        it["mk"].append((identb, mcore[:, kt - 4 * i, :], (t_cb, t_mc), False))
                    if g == 0 and kk == 0 and qd + 2 <= i:
                        it["pre"] = (lambda q=qd + 2: fetch(q))
                    items.append(it)
        def prep_sel(it):
            rs_ = slots[it["qd"]]
            kk = it["kk"]; g = it["g"]
            it["kT"] = kring[rs_][:, kk * 128:(kk + 1) * 128]
            it["vA"] = kring[rs_][:, 512 + kk * 130 + g * 65:512 + kk * 130 + (g + 1) * 65]
            it["masks"] = it["mk"]
            it["rds"] = (t_ring[rs_],)
            it["sbanks"] = (0, 1, 3)

        cad = max(1, len(items) // 52)
        bgc = [0]

        def bgstep():
            bgc[0] += 1
            if bg is not None and bgc[0] % cad == 0:
                next(bg, None)
        run_pipeline(items, depth=2, prep=prep_sel, bgstep=bgstep)
        if bg is not None:
            for _ in bg:
                pass
        for g in range(2):
            finish(g, 6 + g, 1)
            gate_out(g, 6 + g, 1)
        for g in range(2):
            gs = slice(64 * g, 64 * g + 64)
            zinit(2, 0, 260)
            jjs = [jj for jj in range(8) if 4 * i - 4 + jj >= 0]
            items = []
            for jj in jjs:
                kt = 4 * i - 4 + jj
                slot = kt % 16
                masks = [(identb, mcore[:, 4 + jj, :], (t_cb, t_mc), False)]
                items.append(dict(g=g, kT=kwR[:, slot, :], vA=vwR[:, slot, g, :], masks=masks, rds=(t_kw[slot],),
                                  acc=2, last=(jj == jjs[-1]), sbanks=(0, 1, 3, 4)))
            run_pipeline(items, depth=2)
            finish(g, 2, 2)
            gate_out(g, 2, 2)
        V("dve", "bn_stats", (t_on,), (t_st,), st6[:, 0, :], onsa[:, :])
        V("dve", "bn_aggr", (t_st,), (t_st,), mv[:, 0, :], st6[:, 0, :])
        V("dve", "tensor_mul", (t_st,), (t_st,), mv[:, 1, 0:1], mv[:, 0, 0:1], mv[:, 0, 0:1])
        V("dve", "tensor_add", (t_st,), (t_st,), mv[:, 1, 0:1], mv[:, 1, 0:1], mv[:, 0, 1:2])
        act(mv[:, 1, 1:2], mv[:, 1, 0:1], AF.Ln, (t_st,), (t_st,), bias=eps6[:, 0:1])
        act(mv[:, 1, 1:2], mv[:, 1, 1:2], AF.Exp, (t_st,), (t_st,), scale=-0.5)
        V("dve", "scalar_tensor_tensor", (t_on, t_st, t_W), (t_mixn,), mixn[:, 0:512], onsa[:, :], mv[:, 1, 1:2],
          nsag[:, :], ALU.mult, ALU.mult)
        if debug.get("onsa") is not None:
            P.dma(dbg["onsa"][i * 128:(i + 1) * 128, :], onsa[:, :], (t_on,), ())

        mp = i % 2
        for ft in range(8):
            tr(PSb5[:, ft * 128:(ft + 1) * 128], mixn[:, ft * 128:(ft + 1) * 128], identb, (t_mixn, t_cb), (pst[5],))
        act(mixT[mp][:, :, :], PSb5[:, :].rearrange("p (a b) -> p a b", a=8), AF.Copy, (pst[5],), (t_mixT[mp],))
        P.dma(mixT_d[i, :, :], mixT[mp][:, :, :].rearrange("p a b -> p (a b)"), (t_mixT[mp],), (t_mixd[i],))

    t_mixd = [Tok() for _ in range(NOWN)]
    t_x2d = [Tok() for _ in range(NOWN)]
    PSb5 = PS[5][:, :].bitcast(BF16)

    kc = [0]

    def sup_gen(i2):
        for jj in range(4):
            k = kc[0]
            kc[0] += 1
            yield from full_tile(4 * i2 + jj, k)

    for i2 in range(min(2, NSUP)):
        for _ in sup_gen(i2):
            pass
    for i in range(NSUP):
        compress(i)
        bg = sup_gen(i + 2) if i + 2 < NSUP else None
        k = kc[0]
        kc[0] += 1
        own_tile(i, k, bg)
        if bg is not None:
            for _ in bg:
                pass
    if "stopA" in debug:
        P.emit()
        return nc

    P.barrier()
    sb = SB(nc)
    cfB = sb.t([128, 128], F32, "cfB"); cbB = sb.t([128, 128], BF16, "cbB"); t_cB = Tok()
    P.dma(cfB[:, :], c_f32[:, 0:128], (), (t_cB,))
    P.dma(cbB[:, :], c_f32[:, 0:128], (), (t_cB,), eng="pool")
    epsB = sb.t([128, 2], F32, "epsB")
    V("pool", "memset", (), (t_cB,), epsB[:, 0:1], 1e-5)
    onesB = sb.t([128, 2], BF16, "onesB")
    V("pool", "memset", (), (t_cB,), onesB[:, :], 1.0)
    lnc = sb.t([128, 6 * D], F32, "lnc"); t_ln = Tok()
    P.dma(lnc[:, :], ln_bc, (), (t_ln,))
    WB_ = {}
    t_WB = Tok()
    for nm, src in (("out", w_out), ("q", wq), ("o", wo), ("k", wk), ("v", wv)):
        WB_[nm] = sb.t([128, 8, D], BF16, "W" + nm)
        sv = src.rearrange("(kt p) c -> p kt c", p=128)
        for kt in range(0, 8, 2):
            P.dma(WB_[nm][:, kt:kt + 2, :], sv[:, kt:kt + 2, :], (), (t_WB,), eng="pool")
    NBLK_ = (NOWN * 256 + 32 * 512) // 128
    xs_d = nc.dram_tensor("xs_d", [NBLK_ * 128, D], BF16, kind="Internal").ap()
    zt = sb.t([128, D], BF16, "zt"); t_zt = Tok(); t_xsz = Tok()
    V("pool", "memset", (), (t_zt,), zt[:, :], 0.0)
    for zz in range(NBLK_ // 4):
        P.dma(xs_d[zz * 512:(zz + 1) * 512, :].rearrange("(p k) d -> p k d", p=128),
              zt[:, :].unsqueeze(1).broadcast_to([128, 4, D]), (t_zt,), (t_xsz,))
    xsB = [sb.t([128, D], F32, "xsB") for _ in range(2)]; t_xsB = [Tok(), Tok()]
    memT = sb.t([128, 8, 256], BF16, "memT"); t_memT = Tok()
    memKT = sb.t([128, 8, 256], BF16, "memKT"); memV = sb.t([128, 2, D], BF16, "memV"); t_mem = Tok()
    for mt in range(2):
        P.dma(xsB[mt][:, :], mem[mt * 128:(mt + 1) * 128, :], (), (t_xsB[mt],))
        for half in range(2):
            for q in range(4):
                kt = half * 4 + q
                tr(PS[half][:, q * 128:(q + 1) * 128], xsB[mt][:, kt * 128:(kt + 1) * 128], cfB[:, :],
                   (t_xsB[mt], t_cB), (pst[half],))
            act(memT[:, half * 4:half * 4 + 4, mt * 128:(mt + 1) * 128],
                PS[half][:, :].rearrange("p (a b) -> p a b", a=4), AF.Copy, (pst[half],), (t_memT,))
    for ft in range(8):
        bank = 2 + ft % 2
        for kt in range(8):
            mm(PS[bank][:, 0:256], WB_["k"][:, kt, ft * 128:(ft + 1) * 128], memT[:, kt, :], (t_WB, t_memT),
               (pst[bank],), start=(kt == 0), stop=(kt == 7))
        act(memKT[:, ft, :], PS[bank][:, 0:256], AF.Copy, (pst[bank],), (t_mem,))
    for mt in range(2):
        for half in range(2):
            bank = 4 + half
            for kt in range(8):
                mm(PS[bank][:, :], memT[:, kt, mt * 128:(mt + 1) * 128], WB_["v"][:, kt, half * 512:(half + 1) * 512],
                   (t_WB, t_memT), (pst[bank],), start=(kt == 0), stop=(kt == 7))
            V("dve", "tensor_copy", (pst[bank],), (t_mem,), memV[:, mt, half * 512:(half + 1) * 512], PS[bank][:, :])

    mixTt = [sb.t([128, 8, 128], BF16, "mixTt") for _ in range(2)]; t_mx = [Tok(), Tok()]
    y1 = sb.t([128, D], F32, "y1"); t_y1 = Tok()
    x1 = sb.t([128, D], F32, "x1"); t_x1 = Tok()
    x1T = sb.t([128, 8, 128], BF16, "x1T"); t_x1T = Tok()
    qxT = sb.t([128, 8, 128], BF16, "qxT"); t_qx = Tok()
    PTb = sb.t([128, 8, 128], BF16, "PTb"); t_PT = Tok()
    on_ = sb.t([128, D], BF16, "on_"); t_onb = Tok()
    oTb = sb.t([128, 8, 128], BF16, "oTb"); t_oT = Tok()
    y2 = sb.t([128, D], F32, "y2"); t_y2 = Tok()
    x2 = [sb.t([128, D], F32, "x2") for _ in range(2)]; t_x2 = [Tok(), Tok()]
    stB = sb.t([128, 2, 6], F32, "stB"); mvB = sb.t([128, 4], F32, "mvB"); t_stB = Tok()
    rdB = sb.t([128, 4], F32, "rdB"); t_rdB = Tok()
    PSb2 = PS[2][:, :].bitcast(BF16)
    print("phase B SBUF end:", sb.cur)

    def layer_norm(src, t_src, dst, t_dst, gi, stt, mvt, t_s, epst):
        for half in range(2):
            V("dve", "bn_stats", (t_src,), (t_s,), stt[:, half, :], src[:, half * 512:(half + 1) * 512])
        V("dve", "bn_aggr", (t_s,), (t_s,), mvt[:, 0:2], stt[:, :, :].rearrange("p a b -> p (a b)"))
        act(mvt[:, 2:3], mvt[:, 1:2], AF.Ln, (t_s,), (t_s,), bias=epst[:, 0:1])
        act(mvt[:, 2:3], mvt[:, 2:3], AF.Exp, (t_s,), (t_s,), scale=-0.5)
        V("dve", "tensor_scalar", (t_src, t_s), (t_dst,), dst[:, :], src[:, :], mvt[:, 0:1], mvt[:, 2:3],
          ALU.subtract, ALU.mult)
        V("dve", "tensor_mul", (t_dst, t_ln), (t_dst,), dst[:, :], dst[:, :], lnc[:, gi * D:(gi + 1) * D])
        V("dve", "tensor_add", (t_dst, t_ln), (t_dst,), dst[:, :], dst[:, :], lnc[:, (gi + 1) * D:(gi + 2) * D])

    for i in range(NOWN):
        p = i % 2
        P.dma(mixTt[p][:, :, :].rearrange("p a b -> p (a b)"), mixT_d[i, :, :], (t_mixd[i],), (t_mx[p],))
        P.dma(xsB[p][:, :], xown[i * 128:(i + 1) * 128, :], (), (t_xsB[p],))
        for half in range(2):
            for kt in range(8):
                mm(PS[half][:, :], mixTt[p][:, kt, :], WB_["out"][:, kt, half * 512:(half + 1) * 512], (t_mx[p], t_WB),
                   (pst[half],), start=(kt == 0), stop=(kt == 7))
            V("dve", "scalar_tensor_tensor", (t_xsB[p], pst[half]), (t_y1,), y1[:, half * 512:(half + 1) * 512],
              xsB[p][:, half * 512:(half + 1) * 512], ALPHA, PS[half][:, :], ALU.mult, ALU.add)
        layer_norm(y1, t_y1, x1, t_x1, 0, stB, mvB, t_stB, epsB)
        if debug.get("x1") is not None:
            P.dma(dbg["x1"][i * 128:(i + 1) * 128, :], x1[:, :], (t_x1,), ())
        for half in range(2):
            for q in range(4):
                kt = half * 4 + q
                tr(PS[half][:, q * 128:(q + 1) * 128], x1[:, kt * 128:(kt + 1) * 128], cfB[:, :], (t_x1, t_cB),
                   (pst[half],))
            act(x1T[:, half * 4:half * 4 + 4, :], PS[half][:, :].rearrange("p (a b) -> p a b", a=4), AF.Copy,
                (pst[half],), (t_x1T,))
        for ft in range(8):
            bank = 2 + ft // 4
            for kt in range(8):
                mm(PS[bank][:, (ft % 4) * 128:(ft % 4 + 1) * 128], WB_["q"][:, kt, ft * 128:(ft + 1) * 128], x1T[:, kt, :],
                   (t_WB, t_x1T), (pst[bank],), start=(kt == 0), stop=(kt == 7))
        for hb in range(2):
            act(qxT[:, hb * 4:hb * 4 + 4, :], PS[2 + hb][:, :].rearrange("p (a b) -> p a b", a=4), AF.Copy,
                (pst[2 + hb],), (t_qx,), scale=1.0 / 16.0)
        for hd in range(4):
            for mt in range(2):
                r8 = hd * 2 + mt
                bank = 4 + r8 // 4
                for k2 in range(2):
                    ft = hd * 2 + k2
                    mm(PS[bank][:, (r8 % 4) * 128:(r8 % 4 + 1) * 128], memKT[:, ft, mt * 128:(mt + 1) * 128], qxT[:, ft, :],
                       (t_mem, t_qx), (pst[bank],), start=(k2 == 0), stop=(k2 == 1))
        for hb in range(2):
            act(PTb[:, hb * 4:hb * 4 + 4, :], PS[4 + hb][:, :].rearrange("p (a b) -> p a b", a=4), AF.Exp,
                (pst[4 + hb],), (t_PT,))
        for hd in range(4):
            bank = 6 + hd // 2
            for mt in range(2):
                mm(PS[bank][:, (hd % 2) * 256:(hd % 2 + 1) * 256], PTb[:, hd * 2 + mt, :], memV[:, mt, hd * 256:(hd + 1) * 256],
                   (t_PT, t_mem), (pst[bank],), start=(mt == 0), stop=(mt == 1))
        for hd in range(4):
            for mt in range(2):
                mm(PS[0][:, hd:hd + 1], PTb[:, hd * 2 + mt, :], onesB[:, 0:1], (t_PT, t_cB), (pst[0],), start=(mt == 0),
                   stop=(mt == 1))
        V("dve", "reciprocal", (pst[0],), (t_rdB,), rdB[:, :], PS[0][:, 0:4])
        for hd in range(4):
            bank = 6 + hd // 2
            V("dve", "tensor_scalar_mul", (pst[bank], t_rdB), (t_onb,), on_[:, hd * 256:(hd + 1) * 256],
              PS[bank][:, (hd % 2) * 256:(hd % 2 + 1) * 256], rdB[:, hd:hd + 1])
        for ft in range(8):
            tr(PSb2[:, ft * 128:(ft + 1) * 128], on_[:, ft * 128:(ft + 1) * 128], cbB[:, :], (t_onb, t_cB), (pst[2],))
        act(oTb[:, :, :], PSb2[:, :].rearrange("p (a b) -> p a b", a=8), AF.Copy, (pst[2],), (t_oT,))
        for half in range(2):
            for kt in range(8):
                mm(PS[half][:, :], oTb[:, kt, :], WB_["o"][:, kt, half * 512:(half + 1) * 512], (t_oT, t_WB),
                   (pst[half],), start=(kt == 0), stop=(kt == 7))
            V("dve", "scalar_tensor_tensor", (t_x1, pst[half]), (t_y2,), y2[:, half * 512:(half + 1) * 512],
              x1[:, half * 512:(half + 1) * 512], ALPHA, PS[half][:, :], ALU.mult, ALU.add)
        layer_norm(y2, t_y2, x2[p], t_x2[p], 2, stB, mvB, t_stB, epsB)
        P.dma(x2_d[i * 128:(i + 1) * 128, :], x2[p][:, :], (t_x2[p],), (t_x2d[i],))
        if debug.get("x2") is not None:
            P.dma(dbg["x2"][i * 128:(i + 1) * 128, :], x2[p][:, :], (t_x2[p],), ())
    if "stopB" in debug:
        P.emit()
        return nc

    P.barrier()
    sb = SB(nc)
    AXX = mybir.AxisListType.X
    cfC = sb.t([128, 128], F32, "cfC"); t_cC = Tok()
    P.dma(cfC[:, :], c_f32[:, 0:128], (), (t_cC,))
    epsC = sb.t([128, 2], F32, "epsC")
    V("pool", "memset", (), (t_cC,), epsC[:, 0:1], 1e-5)
    ln3 = sb.t([128, 2 * D], F32, "ln3"); t_ln = Tok()
    P.dma(ln3[:, :], ln_bc[:, 4 * D:6 * D], (), (t_ln,))
    Wr = sb.t([128, 8, 36], F32, "Wr"); brt = sb.t([128, 36], F32, "brt"); t_wr = Tok()
    P.dma(Wr[:, :, :], mo_wr.rearrange("(kt p) c -> p kt c", p=128), (), (t_wr,))
    P.dma(brt[:, :], mo_br, (), (t_wr,))
    I32 = mybir.dt.int32
    BS = 256
    NBLK = (NOWN * 256 + 32 * BS) // BS
    NSLOT = NBLK * BS
    ys_d = nc.dram_tensor("ys_d", [NSLOT, D], F32, kind="Internal").ap()
    cbC = sb.t([128, 3, 128], BF16, "cbC")
    P.dma(cbC[:, 0, :], c_f32[:, 0:128], (), (t_cC,), eng="pool")
    P.dma(cbC[:, 1, :], c_moe[:, 0:128], (), (t_cC,), eng="pool")
    V("pool", "memset", (), (t_cC,), cbC[:, 2, :], 1.0)
    cmo = sb.t([128, 12 + 192], F32, "cmo"); t_cmo = Tok()
    P.dma(cmo[:, :], c_moe[:, 128:128 + 12 + 192], (), (t_cmo,))
    base_rel = sb.t([128, 32], F32, "base_rel"); t_base = Tok()
    V("pool", "memset", (), (t_base,), base_rel[:, :], 0.0)
    oh_all = sb.t([128, NOWN, 2, 32], F32, "oh_all"); rank_all = sb.t([128, NOWN, 2], F32, "rank_all")
    dest_f = sb.t([128, NOWN, 2], F32, "dest_f")
    dest_i = sb.t([128, NOWN, 2], I32, "dest_i"); wts = sb.t([128, NOWN, 2], F32, "wts")
    t_dst = [Tok() for _ in range(NOWN)]
    xsC = [sb.t([128, D], F32, "xsC") for _ in range(2)]; t_xsC = [Tok(), Tok()]
    x2b = [sb.t([128, D], BF16, "x2b") for _ in range(2)]; t_x2b = [Tok(), Tok()]
    x2Tf = sb.t([128, 8, 128], F32, "x2Tf"); t_x2f = Tok()
    lg = sb.t([128, 36], F32, "lg"); elm = sb.t([128, 32], F32, "elm"); ohx = sb.t([128, 2, 32], F32, "ohx")
    ohs = sb.t([128, 32], BF16, "ohs"); pos = sb.t([128, 32], F32, "pos")
    rs = sb.t([128, 20], F32, "rs"); t_r = Tok()
    for tg in range(NOWN):
        b_ = tg % 2
        P.dma(xsC[b_][:, :], x2_d[tg * 128:(tg + 1) * 128, :], (t_x2d[tg],), (t_xsC[b_],))
        for half in range(2):
            for q in range(4):
                kt = half * 4 + q
                tr(PS[half][:, q * 128:(q + 1) * 128], xsC[b_][:, kt * 128:(kt + 1) * 128], cfC[:, :],
                   (t_xsC[b_], t_cC), (pst[half],))
            V("dve", "tensor_copy", (pst[half],), (t_x2f,), x2Tf[:, half * 4:half * 4 + 4, :],
              PS[half][:, :].rearrange("p (a b) -> p a b", a=4))
        for kt in range(8):
            mm(PS[2][:, 0:36], x2Tf[:, kt, :], Wr[:, kt, :], (t_x2f, t_wr), (pst[2],), start=(kt == 0), stop=(kt == 7))
        V("dve", "tensor_add", (pst[2], t_wr), (t_r,), lg[:, :], PS[2][:, 0:36], brt[:, :])
        V("dve", "reduce_max", (t_r,), (t_r,), rs[:, 0:1], lg[:, 0:4], AXX)
        V("dve", "tensor_scalar_mul", (t_r,), (t_r,), rs[:, 1:2], rs[:, 0:1], -1.0)
        act(ohx[:, 0, 0:4], lg[:, 0:4], AF.Exp, (t_r,), (t_r,), bias=rs[:, 1:2], accum_out=rs[:, 2:3])
        V("dve", "reciprocal", (t_r,), (t_r,), rs[:, 3:4], rs[:, 2:3])
        V("dve", "tensor_scalar", (t_r,), (t_r,), rs[:, 4:8], lg[:, 0:4], rs[:, 0:1], 1.0e30, ALU.is_equal, ALU.mult)
        V("dve", "tensor_scalar_add", (t_r,), (t_r,), rs[:, 4:8], rs[:, 4:8], -1.0e30)
        V("dve", "tensor_add", (t_r,), (t_r,), elm[:, :].rearrange("p (g e) -> p g e", g=4),
          lg[:, 4:36].rearrange("p (g e) -> p g e", g=4), rs[:, 4:8].unsqueeze(2).broadcast_to([128, 4, 8]))
        V("dve", "max", (t_r,), (t_r,), rs[:, 8:16], elm[:, :])
        V("dve", "tensor_sub", (t_r,), (t_r,), rs[:, 2:3], rs[:, 9:10], rs[:, 8:9])
        act(rs[:, 2:3], rs[:, 2:3], AF.Exp, (t_r,), (t_r,))
        V("dve", "tensor_scalar_add", (t_r,), (t_r,), rs[:, 2:3], rs[:, 2:3], 1.0)
        V("dve", "reciprocal", (t_r,), (t_r,), rs[:, 2:3], rs[:, 2:3])
        V("dve", "tensor_mul", (t_r,), (t_dst[tg],), wts[:, tg, 0:1], rs[:, 2:3], rs[:, 3:4])
        V("dve", "tensor_sub", (t_r, t_dst[tg]), (t_dst[tg],), wts[:, tg, 1:2], rs[:, 3:4], wts[:, tg, 0:1])
        V("dve", "tensor_scalar", (t_r,), (t_dst[tg],), oh_all[:, tg, 0, :], elm[:, :], rs[:, 8:9], None, ALU.is_equal)
        V("dve", "tensor_scalar", (t_r,), (t_dst[tg],), oh_all[:, tg, 1, :], elm[:, :], rs[:, 9:10], None, ALU.is_equal)
        V("dve", "tensor_add", (t_dst[tg],), (t_r,), ohs[:, :], oh_all[:, tg, 0, :], oh_all[:, tg, 1, :])
        mm(PS[3][:, 0:32], cbC[:, 1, :], ohs[:, :], (t_cC, t_r), (pst[3],))
        mm(PS[3][:, 32:64], cbC[:, 2, :], ohs[:, :], (t_cC, t_r), (pst[3],))
        V("dve", "tensor_add", (pst[3], t_base), (t_r,), pos[:, :], PS[3][:, 0:32], base_rel[:, :])
        V("dve", "tensor_add", (pst[3], t_base), (t_base,), base_rel[:, :], base_rel[:, :], PS[3][:, 32:64])
        for k2 in range(2):
            V("dve", "tensor_mul", (t_r, t_dst[tg]), (t_r,), ohx[:, k2, :], oh_all[:, tg, k2, :], pos[:, :])
            V("dve", "reduce_sum", (t_r,), (t_dst[tg],), rank_all[:, tg, k2:k2 + 1], ohx[:, k2, :], AXX)
    pad = sb.t([128, 4, 32], F32, "pad"); t_pad = Tok()
    NTH = 8192 // BS
    cm2 = sb.t([128, 32, NTH], F32, "cm2")
    V("dve", "tensor_tensor", (t_base, t_cmo), (t_pad,), cm2[:, :, :], base_rel[:, :].unsqueeze(2).broadcast_to([128, 32, NTH]),
      cmo[:, 12:12 + (BS // 128) * NTH:BS // 128].unsqueeze(1).broadcast_to([128, 32, NTH]), ALU.is_gt)
    V("dve", "reduce_sum", (t_pad,), (t_pad,), pad[:, 0, :], cm2[:, :, :], AXX)
    V("dve", "tensor_scalar_mul", (t_pad,), (t_pad,), pad[:, 1, :], pad[:, 0, :], float(BS))
    V("pool", "memset", (), (t_pad,), pad[:, 3, :], 1.0)
    V("dve", "tensor_tensor_scan", (t_pad,), (t_pad,), pad[:, 2, :], pad[:, 3, :], pad[:, 1, :], 0.0, ALU.mult, ALU.add)
    V("dve", "tensor_sub", (t_pad,), (t_pad,), pad[:, 0, :], pad[:, 2, :], pad[:, 1, :])
    for tg in range(NOWN):
        for k2 in range(2):
            V("dve", "tensor_mul", (t_pad, t_dst[tg]), (t_r,), ohx[:, k2, :], oh_all[:, tg, k2, :], pad[:, 0, :])
            V("dve", "reduce_sum", (t_r,), (t_r,), rs[:, 16 + k2:17 + k2], ohx[:, k2, :], AXX)
        V("dve", "tensor_add", (t_r, t_dst[tg]), (t_dst[tg],), dest_f[:, tg, :], rs[:, 16:18], rank_all[:, tg, :])
        V("dve", "tensor_copy", (t_dst[tg],), (t_dst[tg],), dest_i[:, tg, :], dest_f[:, tg, :])
        if debug.get("wt") is not None:
            P.dma(dbg["wt"][tg * 128:(tg + 1) * 128, 0:2], dest_f[:, tg, :], (t_dst[tg],), ())
            P.dma(dbg["wt"][tg * 128:(tg + 1) * 128, 2:4], wts[:, tg, :], (t_dst[tg],), ())
    cmpb = sb.t([128, NBLK, 32], F32, "cmpb"); bex = sb.t([128, NBLK], F32, "bex"); t_be = Tok()
    V("dve", "tensor_tensor", (t_pad, t_cmo), (t_be,), cmpb[:, :, :], pad[:, 2, :].unsqueeze(1).broadcast_to([128, NBLK, 32]),
      cmo[:, 12:12 + (BS // 128) * NBLK:BS // 128].unsqueeze(2).broadcast_to([128, NBLK, 32]), ALU.is_le)
    V("dve", "reduce_sum", (t_be,), (t_be,), bex[:, :], cmpb[:, :, :], AXX)
    V("dve", "tensor_scalar_min", (t_be,), (t_be,), bex[:, :], bex[:, :], 31.0)
    idxf = sb.t([128, NBLK, 6], F32, "idxf"); idxi = sb.t([128, NBLK, 6], I32, "idxi"); t_idx = Tok()
    V("dve", "scalar_tensor_tensor", (t_be, t_cmo), (t_idx,), idxf[:, :, 0:4], bex[:, :].unsqueeze(2).broadcast_to([128, NBLK, 4]),
      512.0, cmo[:, 0:4].unsqueeze(1).broadcast_to([128, NBLK, 4]), ALU.mult, ALU.add)
    V("dve", "scalar_tensor_tensor", (t_be, t_cmo), (t_idx,), idxf[:, :, 4:6], bex[:, :].unsqueeze(2).broadcast_to([128, NBLK, 2]),
      256.0, cmo[:, 0:2].unsqueeze(1).broadcast_to([128, NBLK, 2]), ALU.mult, ALU.add)
    V("dve", "tensor_copy", (t_idx,), (t_idx,), idxi[:, :, :], idxf[:, :, :])
    t_scat = [Tok() for _ in range(NOWN)]
    for tg in range(NOWN):
        b_ = tg % 2
        P.dma(xsC[b_][:, :], x2_d[tg * 128:(tg + 1) * 128, :], (t_x2d[tg],), (t_xsC[b_],))
        act(x2b[b_][:, :], xsC[b_][:, :], AF.Copy, (t_xsC[b_],), (t_x2b[b_],))
        for k2 in range(2):
            P.op("pool", (lambda e, tg=tg, k2=k2, b_=b_: e.indirect_dma_start(
                out=xs_d[:, :], out_offset=bass.IndirectOffsetOnAxis(ap=dest_i[:, tg, k2:k2 + 1], axis=0),
                in_=x2b[b_][:, :], in_offset=None)),
                (t_x2b[b_], t_dst[tg]), (t_scat[tg],), dma=True)
    WGUb = [sb.t([128, 8, 1024], BF16, "WGUb") for _ in range(2)]
    WDb = [sb.t([128, 4, D], BF16, "WDb") for _ in range(2)]
    t_wgu = [[Tok() for _ in range(4)] for _ in range(2)]
    t_wd = [[Tok() for _ in range(2)] for _ in range(2)]
    xe = [sb.t([128, D], BF16, "xe") for _ in range(2)]; t_xe = [Tok(), Tok()]
    xeT = [sb.t([128, 8, 128], BF16, "xeT") for _ in range(2)]; t_xeT = [Tok(), Tok()]
    sgC = sb.t([128, 512], F32, "sgC"); t_sg = Tok()
    hb = sb.t([128, 512], BF16, "hb"); t_hb = Tok()
    hTb = [sb.t([128, 4, 128], BF16, "hTb") for _ in range(2)]; t_hT = [Tok(), Tok()]
    ysb = [sb.t([128, D], F32, "ysb") for _ in range(2)]; t_ysb = [Tok(), Tok()]
    t_ysd = [Tok() for _ in range(NBLK)]
    PSbC = [PS[6][:, :].bitcast(BF16), PS[7][:, :].bitcast(BF16)]

    def wload(blk):
        wb_ = blk % 2
        for kp in range(4):
            P.op("pool", (lambda e, blk=blk, kp=kp, wb_=wb_: e.indirect_dma_start(
                out=WGUb[wb_][:, 2 * kp:2 * kp + 2, :].rearrange("p a b -> p (a b)"), out_offset=None, in_=mo_wgu[:, :],
                in_offset=bass.IndirectOffsetOnAxis(ap=idxi[:, blk, kp:kp + 1], axis=0))),
                (t_idx,), (t_wgu[wb_][kp],), dma=True)
        for fp in range(2):
            P.op("pool", (lambda e, blk=blk, fp=fp, wb_=wb_: e.indirect_dma_start(
                out=WDb[wb_][:, 2 * fp:2 * fp + 2, :].rearrange("p a b -> p (a b)"), out_offset=None, in_=mo_wd2[:, :],
                in_offset=bass.IndirectOffsetOnAxis(ap=idxi[:, blk, 4 + fp:5 + fp], axis=0))),
                (t_idx,), (t_wd[wb_][fp],), dma=True)

    sg2 = [sgC, sb.t([128, 512], F32, "sgC2")]; t_sg2 = [t_sg, Tok()]
    hb2 = [hb, sb.t([128, 512], BF16, "hb2")]; t_hb2 = [t_hb, Tok()]
    NSTB = BS // 128
    NTIL = NBLK * NSTB
    PSb3 = PS[3][:, :].bitcast(BF16)

    def stA(sidx):
        xb_ = sidx % 2
        P.dma(xe[xb_][:, :], xs_d[sidx * 128:(sidx + 1) * 128, :], tuple(t_scat), (t_xe[xb_],))
        pb_ = 6 + xb_
        for kt in range(8):
            tr(PSbC[xb_][:, kt * 128:(kt + 1) * 128], xe[xb_][:, kt * 128:(kt + 1) * 128], cbC[:, 0, :],
               (t_xe[xb_], t_cC), (pst[pb_],))
        act(xeT[xb_][:, :, :], PSbC[xb_][:, :].rearrange("p (a b) -> p a b", a=8), AF.Copy, (pst[pb_],), (t_xeT[xb_],))

    def stB(sidx):
        blk = sidx // NSTB
        wb_ = blk % 2
        xb_ = sidx % 2
        for kt in range(8):
            mm(PS[0][:, :], xeT[xb_][:, kt, :], WGUb[wb_][:, kt, 0:512], (t_xeT[xb_], t_wgu[wb_][kt // 2]), (pst[0],),
               start=(kt == 0), stop=(kt == 7))
        for kt in range(8):
            mm(PS[1][:, :], xeT[xb_][:, kt, :], WGUb[wb_][:, kt, 512:1024], (t_xeT[xb_], t_wgu[wb_][kt // 2]), (pst[1],),
               start=(kt == 0), stop=(kt == 7))
        act(sg2[xb_][:, :], PS[0][:, :], AF.Silu, (pst[0],), (t_sg2[xb_],))
        V("dve", "tensor_mul", (t_sg2[xb_], pst[1]), (t_hb2[xb_],), hb2[xb_][:, :], sg2[xb_][:, :], PS[1][:, :])
        for ft in range(4):
            tr(PSb3[:, ft * 128:(ft + 1) * 128], hb2[xb_][:, ft * 128:(ft + 1) * 128], cbC[:, 0, :], (t_hb2[xb_], t_cC),
               (pst[3],))
        V("dve", "tensor_copy", (pst[3],), (t_hT[xb_],), hTb[xb_][:, :, :], PSb3[:, 0:512].rearrange("p (a b) -> p a b", a=4))

    def stC(sidx):
        blk = sidx // NSTB
        wb_ = blk % 2
        xb_ = sidx % 2
        for h2 in range(2):
            bank = 4 + h2
            for ft in range(4):
                mm(PS[bank][:, :], hTb[xb_][:, ft, :], WDb[wb_][:, ft, h2 * 512:(h2 + 1) * 512], (t_hT[xb_], t_wd[wb_][ft // 2]),
                   (pst[bank],), start=(ft == 0), stop=(ft == 3))
            if h2 == 0:
                act(ysb[xb_][:, 0:512], PS[bank][:, :], AF.Copy, (pst[bank],), (t_ysb[xb_],))
            else:
                V("dve", "tensor_copy", (pst[bank],), (t_ysb[xb_],), ysb[xb_][:, 512:1024], PS[bank][:, :])
        P.dma(ys_d[sidx * 128:(sidx + 1) * 128, :], ysb[xb_][:, :], (t_ysb[xb_],), (t_ysd[blk],))

    wload(0)
    for t_ in range(NTIL + 2):
        if t_ < NTIL and t_ % NSTB == 0 and t_ // NSTB + 1 < NBLK:
            pass
        if t_ - 2 >= 0:
            stC(t_ - 2)
        if 0 <= t_ - 1 < NTIL:
            if (t_ - 1) % NSTB == 0 and (t_ - 1) // NSTB + 1 < NBLK:
                wload((t_ - 1) // NSTB + 1)
            stB(t_ - 1)
        if t_ < NTIL:
            stA(t_)
    g12 = [sb.t([128, 2, D], F32, "g12") for _ in range(2)]; t_g12 = [Tok(), Tok()]
    y3 = sb.t([128, D], F32, "y3"); t_y3 = Tok()
    o3 = [sb.t([128, D], F32, "o3") for _ in range(2)]; t_o3 = [Tok(), Tok()]
    stC = sb.t([128, 2, 6], F32, "stC"); mvC = sb.t([128, 4], F32, "mvC"); t_stC = Tok()
    print("phase C SBUF end:", sb.cur)
    for tg in range(NOWN):
        b_ = tg % 2
        P.dma(xsC[b_][:, :], x2_d[tg * 128:(tg + 1) * 128, :], (t_x2d[tg],), (t_xsC[b_],))
        for k2 in range(2):
            P.op("pool", (lambda e, tg=tg, k2=k2, b_=b_: e.indirect_dma_start(
                out=g12[b_][:, k2, :], out_offset=None, in_=ys_d[:, :],
                in_offset=bass.IndirectOffsetOnAxis(ap=dest_i[:, tg, k2:k2 + 1], axis=0))),
                tuple(t_ysd) + (t_dst[tg],), (t_g12[b_],), dma=True)
        V("dve", "scalar_tensor_tensor", (t_g12[b_], t_dst[tg]), (t_y3,), y3[:, :], g12[b_][:, 0, :], wts[:, tg, 0:1],
          g12[b_][:, 0, :], ALU.mult, ALU.bypass)
        V("dve", "scalar_tensor_tensor", (t_g12[b_], t_dst[tg], t_y3), (t_y3,), y3[:, :], g12[b_][:, 1, :], wts[:, tg, 1:2],
          y3[:, :], ALU.mult, ALU.add)
        V("dve", "scalar_tensor_tensor", (t_xsC[b_], t_y3), (t_y3,), y3[:, :], xsC[b_][:, :], ALPHA, y3[:, :], ALU.mult,
          ALU.add)
        ob = tg % 2
        for half in range(2):
            V("dve", "bn_stats", (t_y3,), (t_stC,), stC[:, half, :], y3[:, half * 512:(half + 1) * 512])
        V("dve", "bn_aggr", (t_stC,), (t_stC,), mvC[:, 0:2], stC[:, :, :].rearrange("p a b -> p (a b)"))
        act(mvC[:, 2:3], mvC[:, 1:2], AF.Ln, (t_stC,), (t_stC,), bias=epsC[:, 0:1])
        act(mvC[:, 2:3], mvC[:, 2:3], AF.Exp, (t_stC,), (t_stC,), scale=-0.5)
        V("dve", "tensor_scalar", (t_y3, t_stC), (t_o3[ob],), o3[ob][:, :], y3[:, :], mvC[:, 0:1], mvC[:, 2:3],
          ALU.subtract, ALU.mult)
        V("dve", "tensor_mul", (t_o3[ob], t_ln), (t_o3[ob],), o3[ob][:, :], o3[ob][:, :], ln3[:, 0:D])
        V("dve", "tensor_add", (t_o3[ob], t_ln), (t_o3[ob],), o3[ob][:, :], o3[ob][:, :], ln3[:, D:2 * D])
        P.dma(out[tg * 128:(tg + 1) * 128, :], o3[ob][:, :], (t_o3[ob],), ())
    P.emit()
    return nc


_NC_CACHE = {}


def kernel(**inputs):
    x = np.asarray(inputs["x"])
    B, S, _ = x.shape
    if S not in _NC_CACHE:
        _NC_CACHE[S] = build(S)
    nc = _NC_CACHE[S]
    maps = prep(inputs, S)
    res = run_bass_kernel_spmd(nc, maps, core_ids=list(range(8)))
    outp = np.zeros((B, S, D), np.float32)
    ov = outp.reshape(B, S // 512, 4, 128, D)
    for core in range(8):
        b, c = core // 4, core % 4
        ov[b, :, c] = np.asarray(res.results[core]["out"]).reshape(S // 512, 128, D)
    return outp


def _consts():
    s_ = np.arange(128)[:, None]
    t_ = np.arange(128)[None, :]
    same = (s_ // 64) == (t_ // 64)
    ident = np.eye(128, dtype=np.float32)
    msuf = ((s_ > t_) & same).astype(np.float32)
    tri = ((s_ <= t_) & same).astype(np.float32)
    chind = (s_ // 64 == np.arange(2)[None, :]).astype(np.float32)
    c_f32 = np.concatenate([ident, msuf, tri, chind], axis=1)
    nl = np.arange(128)[:, None]
    jl = np.arange(33)[None, :]
    msel = ((4 * jl - 1 <= nl) & (nl <= 4 * jl + 3)).astype(np.float32)
    p = np.arange(128)[:, None, None]
    v = np.arange(64)[None, :, None]
    k = np.arange(128)[None, None, :]
    ex32 = (p == 2 * v + k // 64).astype(np.float32).reshape(128, 64 * 128)
    seg = np.ones((128, 512), np.float32)
    seg[:, ::64] = 0.0
    return c_f32, msel, ex32, seg


def _core_consts(c):
    k = np.arange(128)[:, None]
    tl = np.arange(128)[None, :]
    Z = np.zeros((128, 128), np.float32)
    N = np.full((128, 128), NEG, np.float32)
    caus = np.where(k <= tl, 0.0, NEG).astype(np.float32)
    wst = np.where(k > tl, 0.0, NEG).astype(np.float32)
    cm = [Z if j < c else (caus if j == c else N) for j in range(4)]
    wm = []
    for jj in range(8):
        if jj < c or jj > 4 + c:
            wm.append(N)
        elif jj == c:
            wm.append(wst)
        elif jj == 4 + c:
            wm.append(caus)
        else:
            wm.append(Z)
    cpm = [np.where(16 * k + 31 <= 512 * q + 128 * c + tl, 0.0, NEG).astype(np.float32) for q in range(4)]
    m_core = np.concatenate(cm + wm + cpm, axis=1)
    t = np.arange(128)[:, None]
    cur = 2 * c + (t >= 64)
    j9 = np.arange(-1, 8)[None, :]
    b9 = np.where((j9 == cur) | (j9 == cur - 1), 1.0e4, np.where(j9 > cur, -1.0, 0.0)).astype(np.float32)
    j8 = np.arange(8)[None, :]
    b8 = np.where((j8 == cur) | (j8 == cur - 1) | (j8 == 0), 1.0e4, np.where(j8 > cur, -1.0, 0.0)).astype(np.float32)
    esel = np.zeros((128, 4), np.float32)
    esel[:, c] = 1.0
    return m_core, np.concatenate([b9, b8, esel], axis=1)


def prep(inputs, S):
    f = lambda a: np.ascontiguousarray(np.asarray(a, dtype=np.float32))
    x = f(inputs["x"])
    B = x.shape[0]
    c_f32, msel, ex32, seg = _consts()
    bc = lambda v_: np.ascontiguousarray(np.broadcast_to(v_[None, :], (128, v_.shape[0])))
    lbl = f(inputs["hg_lb_logits"])
    pk = f(inputs["cmp_pe_k"])[0].reshape(16, 2, 64).transpose(1, 2, 0).reshape(128, 16)
    pv = f(inputs["cmp_pe_v"])[0].reshape(16, 2, 64).transpose(1, 2, 0).reshape(128, 16)
    common = {
        "w_in": f(inputs["w_in"])[0],
        "w1k": f(inputs["cmp_w1_k"])[0], "w1v": f(inputs["cmp_w1_v"])[0],
        "w2k": f(inputs["cmp_w2_k"])[0], "w2v": f(inputs["cmp_w2_v"])[0],
        "peT": np.ascontiguousarray(np.concatenate([pk, pv], axis=1)),
        "lbl_bc": np.concatenate([bc(lbl[0]), bc(lbl[1])], axis=1),
        "lbl_km": np.ascontiguousarray(np.concatenate([lbl[0].reshape(4, 128).T, lbl[1].reshape(4, 128).T], axis=1)),
        "nsag_bc": bc(f(inputs["nsa_norm_g"])[0]),
        "hgg_bc": bc(np.tile(f(inputs["hg_norm_g"])[0], 4)),
        "c_f32": c_f32, "c_msel": msel, "c_ex32": ex32, "c_seg": seg,
        "c_moe": np.ascontiguousarray(np.concatenate([
            (np.arange(128)[:, None] < np.arange(128)[None, :]).astype(np.float32),
            (np.arange(8)[None, :] * 128.0 + np.arange(128)[:, None]).astype(np.float32),
            (np.arange(4)[None, :] * 128.0 + np.arange(128)[:, None]).astype(np.float32),
            np.broadcast_to((np.arange(192) * 128.0).astype(np.float32)[None, :], (128, 192))], axis=1)),
        "w_out": f(inputs["w_out"])[0], "xa_wq": f(inputs["xa_wq"])[0], "xa_wk": f(inputs["xa_wk"])[0],
        "xa_wv": f(inputs["xa_wv"])[0], "xa_wo": f(inputs["xa_wo"])[0],
        "ln_bc": np.concatenate([bc(f(inputs[k_])[0]) for k_ in ("ln1_g", "ln1_b", "ln2_g", "ln2_b", "ln3_g", "ln3_b")],
                                axis=1),
        "mo_wr": np.ascontiguousarray(np.concatenate([f(inputs["moe_w_group"])[0], f(inputs["moe_w_expert"])[0]], axis=1)),
        "mo_br": bc(np.concatenate([f(inputs["moe_b_group"])[0], f(inputs["moe_b_expert"])[0]])),
        "mo_wgu": np.ascontiguousarray(np.concatenate([f(inputs["moe_w_gate"])[0], f(inputs["moe_w_up"])[0]], axis=2)
                                       .reshape(32, 4, 2, 128, 1024).transpose(0, 1, 3, 2, 4).reshape(32 * 4 * 128, 2048)),
        "mo_wd2": np.ascontiguousarray(f(inputs["moe_w_down"])[0].reshape(32, 2, 2, 128, 1024).transpose(0, 1, 3, 2, 4)
                                       .reshape(32 * 2 * 128, 2048)),
    }
    maps = []
    for core in range(2 * 4):
        b, c = core // 4, core % 4
        if b >= B:
            b = B - 1
        m_core, v_core = _core_consts(c)
        xb = x[b]
        xo = np.ascontiguousarray(xb.reshape(S // 512, 4, 128, D)[:, c].reshape(-1, D))
        d = dict(common)
        d.update({"xfull": xb, "xown": xo, "mem": f(inputs["mem"])[b], "m_core": m_core, "v_core": v_core})
        maps.append(d)
    return maps
```
